# Optimizing a Trainium2 kernel written in Bass

```python
import math
import jax, jax.numpy as jnp
from jax import lax
import numpy as np

D_MODEL = 2048
BATCH = 8
SEQ = 2048
DEPTH = 1

MEM_LEN = 256
MEM_HEADS = 4
MEM_HEAD_DIM = 128
CHUNK = 128
GMLP_HEADS = 8
GMLP_HEAD_DIM = 128
GMLP_WIDTH = GMLP_HEADS * GMLP_HEAD_DIM
MLA_HEADS = 8
Q_LORA = 512
KV_LORA = 256
QK_NOPE = 128
QK_ROPE = 64
V_DIM = 128
MLA_WIDTH = MLA_HEADS * V_DIM
Q_BLOCK = 128
ROPE_THETA = 10000.0
IN_WIDTH = 2 * GMLP_WIDTH + Q_LORA + KV_LORA + QK_ROPE
MIX_WIDTH = GMLP_WIDTH + MLA_WIDTH
N_GROUPS = 8
EXPERTS_PER_GROUP = 8
N_EXPERTS = N_GROUPS * EXPERTS_PER_GROUP
TOP_K = 2
D_EXPERT = 512
MOE_BLOCK = 128
EPS = 1e-6

kernel_name = "hybrid_gmlp_mla_memxattn_hiermoe"


def rms_norm(x, g):
    xf = x.astype(jnp.float32)
    y = xf * lax.rsqrt(jnp.mean(xf * xf, axis=-1, keepdims=True) + EPS)
    return (y * g.astype(jnp.float32)).astype(x.dtype)


def layer_norm(x, g, b):
    xf = x.astype(jnp.float32)
    mu = jnp.mean(xf, axis=-1, keepdims=True)
    var = jnp.mean(jnp.square(xf - mu), axis=-1, keepdims=True)
    y = (xf - mu) * lax.rsqrt(var + 1e-5)
    return (y * g.astype(jnp.float32) + b.astype(jnp.float32)).astype(x.dtype)


def rotary(t, cos, sin):
    half = t.shape[-1] // 2
    t1, t2 = t[..., :half], t[..., half:]
    return jnp.concatenate([t1 * cos - t2 * sin, t2 * cos + t1 * sin], axis=-1)


def gmlp_group(z_uv, g_v, b_v, w_spatial, b_spatial):
    B, S, _ = z_uv.shape
    uv = jax.nn.gelu(z_uv)
    u, v = uv[..., :GMLP_WIDTH], uv[..., GMLP_WIDTH:]
    v = layer_norm(v, g_v, b_v)
    v = v.reshape(B, S // CHUNK, CHUNK, GMLP_HEADS, GMLP_HEAD_DIM)
    causal = jnp.tril(jnp.ones((CHUNK, CHUNK), dtype=bool))
    ws = jnp.where(causal[None], w_spatial, 0.0).astype(v.dtype)
    sv = jnp.einsum('gts,bnsgc->bntgc', ws, v)
    sv = sv + b_spatial.T.astype(v.dtype)[None, None, :, :, None]
    return u * sv.reshape(B, S, GMLP_WIDTH)


def mla_group(c_q, c_kv, k_rope, g_q_lora, w_uq, g_kv_lora, w_ukv):
    B, S, _ = c_q.shape
    q = (rms_norm(c_q, g_q_lora) @ w_uq).reshape(B, S, MLA_HEADS, QK_NOPE + QK_ROPE)
    q_nope, q_rope = q[..., :QK_NOPE], q[..., QK_NOPE:]
    kv = (rms_norm(c_kv, g_kv_lora) @ w_ukv).reshape(B, S, MLA_HEADS, QK_NOPE + V_DIM)
    k_nope, v = kv[..., :QK_NOPE], kv[..., QK_NOPE:]

    pos = jnp.arange(S, dtype=jnp.float32)
    inv_freq = ROPE_THETA ** (-jnp.arange(0, QK_ROPE, 2, dtype=jnp.float32) / QK_ROPE)
    ang = pos[:, None] * inv_freq[None, :]
    cos, sin = jnp.cos(ang).astype(q.dtype), jnp.sin(ang).astype(q.dtype)
    q_rope = rotary(q_rope, cos[None, :, None, :], sin[None, :, None, :])
    k_rope = rotary(k_rope, cos[None], sin[None])

    nb = S // Q_BLOCK
    scale = 1.0 / math.sqrt(QK_NOPE + QK_ROPE)
    k_idx = jnp.arange(S)

    def to_blocks(t):
        return jnp.moveaxis(t.reshape(B, nb, Q_BLOCK, *t.shape[2:]), 1, 0)

    def attend(args):
        qn, qr, blk = args
        s = (jnp.einsum('bqhd,bkhd->bhqk', qn, k_nope)
             + jnp.einsum('bqhr,bkr->bhqk', qr, k_rope)).astype(jnp.float32) * scale
        q_idx = blk * Q_BLOCK + jnp.arange(Q_BLOCK)
        s = jnp.where(k_idx[None, :] <= q_idx[:, None], s, jnp.finfo(jnp.float32).min)
        p = jax.nn.softmax(s, axis=-1).astype(v.dtype)
        return jnp.einsum('bhqk,bkhd->bqhd', p, v)

    o = lax.map(attend, (to_blocks(q_nope), to_blocks(q_rope), jnp.arange(nb)))
    return jnp.moveaxis(o, 0, 1).reshape(B, S, MLA_WIDTH)


def memory_cross_attention(h, mem_n, w_mq, w_mk, w_mv, w_mo):
    B, S, _ = h.shape
    q = (h @ w_mq).reshape(B, S, MEM_HEADS, MEM_HEAD_DIM)
    k = (mem_n @ w_mk).reshape(B, -1, MEM_HEADS, MEM_HEAD_DIM)
    v = (mem_n @ w_mv).reshape(B, -1, MEM_HEADS, MEM_HEAD_DIM)
    s = jnp.einsum('bshd,bmhd->bhsm', q, k).astype(jnp.float32) / math.sqrt(MEM_HEAD_DIM)
    p = jax.nn.softmax(s, axis=-1).astype(v.dtype)
    o = jnp.einsum('bhsm,bmhd->bshd', p, v).reshape(B, S, MEM_HEADS * MEM_HEAD_DIM)
    return o @ w_mo


def hierarchical_moe(h, w_rg, b_rg, w_re, b_re, w_gate, w_up, w_down):
    B, S, D = h.shape
    T = B * S
    ht = h.reshape(T, D)
    g_logits = (ht @ w_rg + b_rg).astype(jnp.float32)
    g_prob = jax.nn.softmax(g_logits, axis=-1)
    grp = jnp.argmax(g_logits, axis=-1)
    p_grp = jnp.take_along_axis(g_prob, grp[:, None], axis=-1)
    e_logits = (ht @ w_re + b_re).astype(jnp.float32).reshape(T, N_GROUPS, EXPERTS_PER_GROUP)
    e_logits = jnp.take_along_axis(e_logits, grp[:, None, None], axis=1)[:, 0]
    e_prob = jax.nn.softmax(e_logits, axis=-1)
    top_p, top_i = lax.top_k(e_prob, TOP_K)
    top_p = top_p / jnp.sum(top_p, axis=-1, keepdims=True)
    gates = (p_grp * top_p).astype(h.dtype)
    expert_idx = grp[:, None] * EXPERTS_PER_GROUP + top_i

    A = T * TOP_K
    P = A + N_EXPERTS * MOE_BLOCK
    NB = P // MOE_BLOCK
    flat_e = expert_idx.reshape(A).astype(jnp.int32)
    flat_tok = jnp.repeat(jnp.arange(T, dtype=jnp.int32), TOP_K)
    flat_w = gates.reshape(A)
    order = jnp.argsort(flat_e)
    sorted_e = flat_e[order]
    counts = jnp.bincount(flat_e, length=N_EXPERTS)
    starts = jnp.cumsum(counts) - counts
    padded = ((counts + MOE_BLOCK - 1) // MOE_BLOCK) * MOE_BLOCK
    padded_ends = jnp.cumsum(padded)
    padded_starts = padded_ends - padded
    dest = padded_starts[sorted_e] + (jnp.arange(A) - starts[sorted_e])
    rows_tok = jnp.full((P,), T, jnp.int32).at[dest].set(flat_tok[order])
    rows_w = jnp.zeros((P,), h.dtype).at[dest].set(flat_w[order])
    block_expert = jnp.clip(jnp.searchsorted(padded_ends, jnp.arange(NB) * MOE_BLOCK, side='right'),
                            0, N_EXPERTS - 1)
    h_pad = jnp.concatenate([ht, jnp.zeros((1, D), ht.dtype)], axis=0)
    xs = h_pad[rows_tok].reshape(NB, MOE_BLOCK, D)

    def expert_block(args):
        xb, e = args
        return (jax.nn.silu(xb @ w_gate[e]) * (xb @ w_up[e])) @ w_down[e]

    ys = lax.map(expert_block, (xs, block_expert)).reshape(P, D) * rows_w[:, None]
    out = jnp.zeros((T + 1, D), h.dtype).at[rows_tok].add(ys)[:T]
    return out.reshape(B, S, D)


def setup_inputs(seed: int = 0) -> dict:
    key = jax.random.key(seed)
    ks = iter(jax.random.split(key, 40))

    def nrm(shape, scale):
        return jax.random.normal(next(ks), shape, jnp.float32) * scale

    def gain(n):
        return 1.0 + 0.05 * jax.random.normal(next(ks), (n,), jnp.float32)

    D = D_MODEL
    return {
        "x": nrm((BATCH, SEQ, D), 1.0),
        "mem": nrm((BATCH, MEM_LEN, D), 1.0),
        "g_norm_mix": gain(D),
        "w_in": nrm((D, IN_WIDTH), D ** -0.5),
        "g_v": gain(GMLP_WIDTH),
        "b_v": nrm((GMLP_WIDTH,), 0.02),
        "w_spatial": nrm((GMLP_HEADS, CHUNK, CHUNK), 0.5 * CHUNK ** -0.5),
        "b_spatial": 1.0 + nrm((GMLP_HEADS, CHUNK), 0.02),
        "g_q_lora": gain(Q_LORA),
        "w_uq": nrm((Q_LORA, MLA_HEADS * (QK_NOPE + QK_ROPE)), Q_LORA ** -0.5),
        "g_kv_lora": gain(KV_LORA),
        "w_ukv": nrm((KV_LORA, MLA_HEADS * (QK_NOPE + V_DIM)), KV_LORA ** -0.5),
        "g_out_gmlp": gain(GMLP_WIDTH),
        "g_out_mla": gain(MLA_WIDTH),
        "w_out": nrm((MIX_WIDTH, D), MIX_WIDTH ** -0.5),
        "g_norm_xattn": gain(D),
        "g_norm_mem": gain(D),
        "w_mq": nrm((D, MEM_HEADS * MEM_HEAD_DIM), D ** -0.5),
        "w_mk": nrm((D, MEM_HEADS * MEM_HEAD_DIM), D ** -0.5),
        "w_mv": nrm((D, MEM_HEADS * MEM_HEAD_DIM), D ** -0.5),
        "w_mo": nrm((MEM_HEADS * MEM_HEAD_DIM, D), (MEM_HEADS * MEM_HEAD_DIM) ** -0.5),
        "g_norm_moe": gain(D),
        "w_router_group": nrm((D, N_GROUPS), D ** -0.5),
        "b_router_group": nrm((N_GROUPS,), 0.01),
        "w_router_expert": nrm((D, N_EXPERTS), D ** -0.5),
        "b_router_expert": nrm((N_EXPERTS,), 0.01),
        "w_exp_gate": nrm((N_EXPERTS, D, D_EXPERT), D ** -0.5),
        "w_exp_up": nrm((N_EXPERTS, D, D_EXPERT), D ** -0.5),
        "w_exp_down": nrm((N_EXPERTS, D_EXPERT, D), D_EXPERT ** -0.5),
        "g_final": gain(D),
    }


def reference(x, mem, g_norm_mix, w_in, g_v, b_v, w_spatial, b_spatial, g_q_lora, w_uq,
              g_kv_lora, w_ukv, g_out_gmlp, g_out_mla, w_out, g_norm_xattn, g_norm_mem,
              w_mq, w_mk, w_mv, w_mo, g_norm_moe, w_router_group, b_router_group,
              w_router_expert, b_router_expert, w_exp_gate, w_exp_up, w_exp_down, g_final):
    c1 = 2 * GMLP_WIDTH
    c2 = c1 + Q_LORA
    c3 = c2 + KV_LORA
    for _ in range(DEPTH):
        z = rms_norm(x, g_norm_mix) @ w_in
        a = gmlp_group(z[..., :c1], g_v, b_v, w_spatial, b_spatial)
        m = mla_group(z[..., c1:c2], z[..., c2:c3], z[..., c3:], g_q_lora, w_uq, g_kv_lora, w_ukv)
        merged = jnp.concatenate([rms_norm(a, g_out_gmlp), rms_norm(m, g_out_mla)], axis=-1)
        x = x + merged @ w_out
        x = x + memory_cross_attention(rms_norm(x, g_norm_xattn), rms_norm(mem, g_norm_mem),
                                       w_mq, w_mk, w_mv, w_mo)
        x = x + hierarchical_moe(rms_norm(x, g_norm_moe), w_router_group, b_router_group,
                                 w_router_expert, b_router_expert, w_exp_gate, w_exp_up, w_exp_down)
    return rms_norm(x, g_final)
```

```python
import contextlib
import math
import numpy as np
import concourse.bass as bass
import concourse.mybir as mybir
from concourse.bass_utils import run_bass_kernel_spmd

F32 = mybir.dt.float32
BF16 = mybir.dt.bfloat16
I32 = mybir.dt.int32
AF = mybir.ActivationFunctionType
ALU = mybir.AluOpType

D = 2048
S = 2048
NT = 16
NEXP = 64
CAP = 256
NBLK = CAP // 128
NSLOT = NEXP * CAP + 128
EPS = 1e-6
ENGS = ("pe", "act", "dve", "pool", "sp")
BLK = {"pe": "tensor", "act": "scalar", "dve": "vector", "pool": "gpsimd", "sp": "sync"}


class Buf:
    __slots__ = ("name", "w", "r", "small")

    def __init__(self, name="", small=False):
        self.name = name
        self.w = None
        self.r = []
        self.small = small


def bufs(n):
    return [Buf() for _ in range(n)]


def sbufs(n):
    return [Buf(small=True) for _ in range(n)]


class Prog:
    NOSYNC_SAME = ("pe", "sp")

    def __init__(self, nc, block, stack):
        self.nc = nc
        self.block = block
        self.eng = {"pe": nc.tensor, "act": nc.scalar, "dve": nc.vector, "pool": nc.gpsimd, "sp": nc.sync}
        self.sem = {e: stack.enter_context(nc.semaphore("s_" + e)) for e in ENGS}
        self.cnt = {e: 0 for e in ENGS}
        self.q = {e: [] for e in ENGS}
        self.seen = {e: {} for e in ENGS}
        self.dsem = {}
        self.stack = stack

    def _semh(self, key):
        return self.sem[key] if key in self.sem else self.dsem[key][0]

    def _deps(self, e, reads, writes):
        need = {}

        def add(dep, small):
            if dep is None:
                return
            k, v = dep
            if k == e and (e in ("pe", "sp") or (e in self.NOSYNC_SAME and not small)):
                return
            if need.get(k, 0) < v:
                need[k] = v
        for b in reads:
            add(b.w, b.small)
        for b in writes:
            add(b.w, b.small)
            for d in b.r:
                add(d, b.small)
        out = []
        for k, v in need.items():
            if self.seen[e].get(k, 0) < v:
                self.seen[e][k] = v
                out.append((k, v))
        return out

    def op(self, e, fn, reads=(), writes=(), signal=True):
        waits = self._deps(e, reads, writes)
        val = self.cnt[e] + 1
        if signal:
            self.cnt[e] = val
        me = (e, val)
        for b in reads:
            b.r.append(me)
        for b in writes:
            b.w = me
            b.r = []
        self.q[e].append((waits, fn, signal, None))

    def dma(self, e, fn, semname, reads=(), writes=()):
        if semname not in self.dsem:
            self.dsem[semname] = [self.stack.enter_context(self.nc.semaphore("d_" + semname)), 0]
        waits = self._deps(e, reads, writes)
        self.dsem[semname][1] += 16
        me = (semname, self.dsem[semname][1])
        for b in reads:
            b.r.append(me)
        for b in writes:
            b.w = me
            b.r = []
        self.q[e].append((waits, fn, False, semname))

    def barrier(self):
        targets = [(k, self.cnt[k]) for k in ENGS if self.cnt[k] > 0]
        targets += [(k, v[1]) for k, v in self.dsem.items() if v[1] > 0]
        for e in ENGS:
            waits = []
            for k, v in targets:
                if k == e:
                    continue
                if self.seen[e].get(k, 0) < v:
                    self.seen[e][k] = v
                    waits.append((k, v))
            if waits:
                self.q[e].append((waits, None, False, None))

    def flush(self):
        for e in ENGS:
            items = self.q[e]
            if not items:
                continue
            self.q[e] = []
            semE = self.sem[e]

            def body(eng, items=items, semE=semE):
                for waits, fn, signal, dsem in items:
                    for k, v in waits:
                        eng.wait_ge(self._semh(k), v)
                    if fn is None:
                        continue
                    ins = fn(eng)
                    if dsem is not None:
                        ins.then_inc(self.dsem[dsem][0], 16)
                    elif signal:
                        ins.then_inc(semE, 1)
            getattr(self.block, BLK[e])(body)


def build_program(debug=False):
    nc = bass.Bass("TRN2", target_bir_lowering=False)

    def din(name, shape, dt=F32):
        return nc.dram_tensor(name, list(shape), dt, kind="ExternalInput").ap()

    x = din("x", [S, D])
    xT = din("xT", [NT, 128, 16, 128])
    mem = din("mem", [256, D])
    g1T = din("g1T", [128, 16])
    w_in = din("w_in", [D, 2880])
    gvb = din("gvb", [128, 1024]); bvb = din("bvb", [128, 1024]); gab = din("gab", [128, 1024])
    wsT = din("wsT", [128, 8, 128]); bspT = din("bspT", [128, 8])
    gqb = din("gqb", [128, 512]); gkvb = din("gkvb", [128, 256])
    w_uq = din("w_uq", [512, 8, 256])
    w_ukv = din("w_ukv", [256, 2048])
    gm = din("gm", [128, 8])
    w_out = din("w_out", [D, D])
    gxb = din("gxb", [128, D]); gmemb = din("gmemb", [128, D]); gmoeb = din("gmoeb", [128, D]); gfb = din("gfb", [128, D])
    w_mq = din("w_mq", [D, 512]); w_mk = din("w_mk", [D, 512]); w_mv = din("w_mv", [D, 512]); w_mo = din("w_mo", [512, D])
    wr = din("wr", [D, 72]); brb = din("brb", [128, 72])
    w_g = din("w_g", [NEXP, 128, 16 * 512]); w_u = din("w_u", [NEXP, 128, 16 * 512]); w_d = din("w_d", [NEXP, 128, 4 * D])
    cs_tok = din("cs_tok", [S, 64]); sn_tok = din("sn_tok", [S, 64])
    csT = din("csT", [64, S]); snT = din("snT", [64, S])
    consts = din("consts", [128, 5, 128])
    iota64 = din("iota64", [128, 64]); dummy = din("dummyrow", [128, 1])
    y = nc.dram_tensor("y", [S, D], F32, kind="ExternalOutput").ap()
    skind = "ExternalOutput" if debug else "Internal"
    X1 = nc.dram_tensor("X1", [S, D], F32, kind=skind).ap()
    X2 = nc.dram_tensor("X2", [S, D], F32, kind=skind).ap()
    Xbuf = nc.dram_tensor("Xbuf", [NSLOT, D], BF16, kind="Internal").ap()
    Ybuf = nc.dram_tensor("Ybuf", [NSLOT, D], BF16, kind="Internal").ap()
    if debug:
        dbgM = nc.dram_tensor("dbgM", [128, 16, S], BF16, kind="ExternalOutput").ap()
        dbgS = nc.dram_tensor("dbgS", [128, NT, 4], F32, kind="ExternalOutput").ap()

    with contextlib.ExitStack() as top:
        def sbt(st, name, shape, dt):
            return st.enter_context(nc.sbuf_tensor(name, list(shape), dt))

        banks = [top.enter_context(nc.psum_tensor("pb%d" % i, [128, 512], F32)) for i in range(8)]
        PB = bufs(8)
        cst = sbt(top, "cst", [128, 5, 128], F32)
        identb = sbt(top, "identb", [128, 128], BF16)
        maskb = sbt(top, "maskb", [128, 128], BF16)
        ltrib = sbt(top, "ltrib", [128, 128], BF16)
        onesb = sbt(top, "onesb", [128, 128], BF16)
        rstd1 = sbt(top, "rstd1", [128, NT], F32)
        slots = sbt(top, "slots", [128, NT, 2], I32)
        gates = sbt(top, "gates", [128, NT, 2], F32)
        neghalf = sbt(top, "neghalf", [128, 1], F32)
        Bnh = Buf()
        block = top.enter_context(nc.Block())
        P = Prog(nc, block, top)
        Bcst, Bident, Bmask, Bltri, Bones = bufs(5)
        Brstd1 = sbufs(NT)
        BaT = bufs(NT)
        BmT = bufs(8 * 4)
        Bslots = sbufs(NT)
        BX1 = bufs(NT); BX2 = bufs(NT); BXb = Buf(); BYb = Buf()

        identf = cst[:, 0, :]
        mid = contextlib.ExitStack()
        aT = sbt(mid, "aT", [128, 8, S], BF16)

        def bank_bf(i):
            return banks[i][:].bitcast(BF16)

        P.dma("sp", lambda e: e.dma_start(out=cst[:], in_=consts[:]), "cst", writes=[Bcst])
        P.op("dve", lambda e: e.tensor_copy(out=identb[:], in_=cst[:, 0, :]), reads=[Bcst], writes=[Bident])
        P.op("dve", lambda e: e.tensor_copy(out=maskb[:], in_=cst[:, 1, :]), reads=[Bcst], writes=[Bmask])
        P.op("dve", lambda e: e.tensor_copy(out=ltrib[:], in_=cst[:, 2, :]), reads=[Bcst], writes=[Bltri])
        P.op("dve", lambda e: e.tensor_copy(out=onesb[:], in_=cst[:, 3, :]), reads=[Bcst], writes=[Bones])
        P.op("dve", lambda e: e.memset(neghalf[:], -0.5), writes=[Bnh])

        def rstd_from_ss(st_tiles, ss_ap, Bss, out_ap, Bout, n, eps):
            tmp, Btmp = st_tiles
            P.op("pool", lambda e: e.tensor_scalar(out=tmp, in0=ss_ap, scalar1=1.0 / n, scalar2=eps, op0=ALU.mult, op1=ALU.add),
                 reads=[Bss], writes=[Btmp])
            P.op("pool", lambda e: e.tensor_tensor(out=out_ap, in0=tmp, in1=neghalf[:, 0:1], op=ALU.pow), reads=[Btmp, Bnh], writes=[Bout])

        def cast_load(dst, src, sem, Bdst):
            P.dma("pool", lambda e: e.dma_start(out=dst, in_=src), sem, writes=[Bdst])

        with contextlib.ExitStack() as st:
            w_uv = sbt(st, "w_uv", [128, 16, 2048], BF16)
            Bwuv = bufs(4)
            wv = w_in.rearrange("(kc p) n -> p kc n", p=128)
            for cb in range(4):
                for hh in range(2):
                    cast_load(w_uv[:, hh * 8:(hh + 1) * 8, cb * 512:(cb + 1) * 512], wv[:, hh * 8:(hh + 1) * 8, cb * 512:(cb + 1) * 512], "wuv%d" % cb, Bwuv[cb])
            g1 = sbt(st, "g1", [128, 16], F32); Bg1 = Buf()
            gv_t = sbt(st, "gv_t", [128, 1024], F32); bv_t = sbt(st, "bv_t", [128, 1024], F32); ga_t = sbt(st, "ga_t", [128, 1024], F32)
            wsf = sbt(st, "wsf", [128, 8, 128], F32); wsb = sbt(st, "wsb", [128, 8, 128], BF16); bsp = sbt(st, "bsp", [128, 8], F32)
            Bgv, Bbv, Bga, Bwsf, Bwsb, Bbsp = bufs(6)
            P.dma("sp", lambda e: e.dma_start(out=g1[:], in_=g1T[:]), "c0", writes=[Bg1])
            P.dma("sp", lambda e: e.dma_start(out=gv_t[:], in_=gvb[:]), "c1", writes=[Bgv])
            P.dma("sp", lambda e: e.dma_start(out=bv_t[:], in_=bvb[:]), "c2", writes=[Bbv])
            P.dma("sp", lambda e: e.dma_start(out=ga_t[:], in_=gab[:]), "c3", writes=[Bga])
            P.dma("sp", lambda e: e.dma_start(out=wsf[:], in_=wsT[:]), "c4", writes=[Bwsf])
            P.dma("sp", lambda e: e.dma_start(out=bsp[:], in_=bspT[:]), "c5", writes=[Bbsp])
            P.op("dve", lambda e: e.tensor_tensor(out=wsb[:], in0=wsf[:], in1=cst[:, 1, :].unsqueeze(1).to_broadcast([128, 8, 128]), op=ALU.mult),
                 reads=[Bwsf, Bcst], writes=[Bwsb])
            xt = sbt(st, "xt0", [128, D], F32); Bxt = Buf()
            xTt = [sbt(st, "xTt%d" % k, [128, 16, 128], F32) for k in range(2)]
            xgT = [sbt(st, "xgT%d" % k, [128, 16, 128], BF16) for k in range(2)]
            BxTt, BxgT = bufs(2), bufs(2)
            u = [sbt(st, "u%d" % k, [128, 1024], BF16) for k in range(2)]
            v = [sbt(st, "v%d" % k, [128, 1024], F32) for k in range(2)]
            vn = [sbt(st, "vn%d" % k, [128, 1024], BF16) for k in range(2)]
            junk = sbt(st, "junk", [128, D], BF16); Bjunk = Buf()
            stt = [sbt(st, "stt%d" % k, [128, 16], F32) for k in range(2)]
            Bu, Bv, Bvn = bufs(2), bufs(2), bufs(2)
            Bst2 = [sbufs(16), sbufs(16)]

            def a1_pre(i):
                k = i % 2
                st_, Bst = stt[k], Bst2[k]
                P.dma("sp", lambda e, i=i: e.dma_start(out=xt[:], in_=x[i * 128:(i + 1) * 128, :]), "xt0", writes=[Bxt])
                P.dma("sp", lambda e, i=i, k=k: e.dma_start(out=xTt[k][:], in_=xT[i]), "xTt%d" % k, writes=[BxTt[k]])
                P.op("act", lambda e: e.activation(out=junk[:], in_=xt[:], func=AF.Square, accum_out=st_[:, 0:1]), reads=[Bxt], writes=[Bjunk, Bst[0]])
                rstd_from_ss((st_[:, 1:2], Bst[1]), st_[:, 0:1], Bst[0], rstd1[:, i:i + 1], Brstd1[i], D, EPS)
                P.op("dve", lambda e, k=k: e.tensor_tensor(out=xgT[k][:], in0=xTt[k][:], in1=g1[:, :].unsqueeze(2).to_broadcast([128, 16, 128]), op=ALU.mult),
                     reads=[BxTt[k], Bg1], writes=[BxgT[k]])

            def a1_mm_pe(i, cbs):
                k = i % 2
                for cb in cbs:
                    bk = cb
                    for kc in range(16):
                        P.op("pe", lambda e, k=k, kc=kc, cb=cb, bk=bk: e.matmul(banks[bk][:], xgT[k][:, kc, :], w_uv[:, kc, cb * 512:(cb + 1) * 512], start=(kc == 0), stop=(kc == 15)),
                             reads=[BxgT[k], Bwuv[cb]], writes=[PB[bk]], signal=(kc == 15))

            def a1_gelu(i):
                k = i % 2
                st_, Bst = stt[k], Bst2[k]
                for cb in range(4):
                    bk = cb
                    if cb < 2:
                        P.op("act", lambda e, cb=cb, bk=bk, i=i, k=k: e.activation(out=u[k][:, cb * 512:(cb + 1) * 512], in_=banks[bk][:], func=AF.Gelu_apprx_tanh, scale=rstd1[:, i:i + 1]),
                             reads=[PB[bk], Brstd1[i]], writes=[Bu[k]])
                    else:
                        c2 = cb - 2
                        P.op("act", lambda e, c2=c2, bk=bk, i=i, k=k: e.activation(out=v[k][:, c2 * 512:(c2 + 1) * 512], in_=banks[bk][:], func=AF.Gelu_apprx_tanh, scale=rstd1[:, i:i + 1],
                                                                              accum_out=st_[:, 2 + c2:3 + c2]),
                             reads=[PB[bk], Brstd1[i]], writes=[Bv[k], Bst[2 + c2]])

            def a1_tail1(i):
                k = i % 2
                st_, Bst = stt[k], Bst2[k]
                vk, uk, vnk = v[k], u[k], vn[k]
                P.op("act", lambda e: e.activation(out=junk[:, 0:1024], in_=vk[:], func=AF.Square, accum_out=st_[:, 4:5]), reads=[Bv[k]], writes=[Bjunk, Bst[4]])
                P.op("dve", lambda e: e.tensor_scalar(out=st_[:, 5:6], in0=st_[:, 2:3], scalar1=st_[:, 3:4], scalar2=1.0 / 1024, op0=ALU.add, op1=ALU.mult),
                     reads=[Bst[2], Bst[3]], writes=[Bst[5]])
                P.op("dve", lambda e: e.tensor_tensor(out=st_[:, 6:7], in0=st_[:, 5:6], in1=st_[:, 5:6], op=ALU.mult), reads=[Bst[5]], writes=[Bst[6]])
                P.op("dve", lambda e: e.scalar_tensor_tensor(out=st_[:, 7:8], in0=st_[:, 4:5], scalar=1.0 / 1024, in1=st_[:, 6:7], op0=ALU.mult, op1=ALU.subtract),
                     reads=[Bst[4], Bst[6]], writes=[Bst[7]])
                P.op("dve", lambda e: e.tensor_scalar(out=st_[:, 8:9], in0=st_[:, 7:8], scalar1=1e-5, scalar2=None, op0=ALU.add), reads=[Bst[7]], writes=[Bst[8]])
                P.op("pool", lambda e: e.tensor_tensor(out=st_[:, 9:10], in0=st_[:, 8:9], in1=neghalf[:, 0:1], op=ALU.pow), reads=[Bst[8], Bnh], writes=[Bst[9]])
                P.op("dve", lambda e: e.tensor_scalar(out=vk[:], in0=vk[:], scalar1=st_[:, 5:6], scalar2=st_[:, 9:10], op0=ALU.subtract, op1=ALU.mult),
                     reads=[Bv[k], Bst[5], Bst[9]], writes=[Bv[k]])
                P.op("dve", lambda e: e.tensor_tensor(out=vk[:], in0=vk[:], in1=gv_t[:], op=ALU.mult), reads=[Bv[k], Bgv], writes=[Bv[k]])
                P.op("dve", lambda e: e.tensor_tensor(out=vnk[:], in0=vk[:], in1=bv_t[:], op=ALU.add), reads=[Bv[k], Bbv], writes=[Bvn[k]])
                for g in range(8):
                    bk = 4 + g // 4
                    P.op("pe", lambda e, g=g, bk=bk: e.matmul(banks[bk][:, (g % 4) * 128:(g % 4 + 1) * 128], wsb[:, g, :], vnk[:, g * 128:(g + 1) * 128], start=True, stop=True),
                         reads=[Bwsb, Bvn[k]], writes=[PB[bk]], signal=(g % 4 == 3))

            def a1_tail2(i):
                k = i % 2
                st_, Bst = stt[k], Bst2[k]
                vk, uk, vnk = v[k], u[k], vn[k]
                for g in range(8):
                    bk = 4 + g // 4
                    P.op("dve", lambda e, g=g, bk=bk: e.scalar_tensor_tensor(out=vk[:, g * 128:(g + 1) * 128], in0=banks[bk][:, (g % 4) * 128:(g % 4 + 1) * 128],
                                                                            scalar=bsp[:, g:g + 1], in1=uk[:, g * 128:(g + 1) * 128], op0=ALU.add, op1=ALU.mult),
                         reads=[PB[bk], Bbsp, Bu[k]], writes=[Bv[k]])
                P.op("act", lambda e: e.activation(out=junk[:, 0:1024], in_=vk[:], func=AF.Square, accum_out=st_[:, 10:11]), reads=[Bv[k]], writes=[Bjunk, Bst[10]])
                rstd_from_ss((st_[:, 11:12], Bst[11]), st_[:, 10:11], Bst[10], st_[:, 12:13], Bst[12], 1024, EPS)
                P.op("dve", lambda e: e.scalar_tensor_tensor(out=vnk[:], in0=vk[:], scalar=st_[:, 12:13], in1=ga_t[:], op0=ALU.mult, op1=ALU.mult),
                     reads=[Bv[k], Bst[12], Bga], writes=[Bvn[k]])
                for c in range(8):
                    P.op("pe", lambda e, c=c: e.transpose(out=bank_bf(6)[:, c * 128:(c + 1) * 128], in_=vnk[:, c * 128:(c + 1) * 128], identity=identb[:]),
                         reads=[Bvn[k], Bident], writes=[PB[6]], signal=(c == 7))
                P.op("act", lambda e, i=i: e.activation(out=aT[:, :, i * 128:(i + 1) * 128], in_=bank_bf(6).rearrange("p (c t) -> p c t", c=8), func=AF.Copy),
                     reads=[PB[6]], writes=[BaT[i]])

            a1_pre(0)
            a1_mm_pe(0, range(4))
            a1_gelu(0)
            a1_pre(1)
            for i in range(NT):
                if i + 1 < NT:
                    a1_mm_pe(i + 1, (0, 1))
                a1_tail1(i)
                if i + 1 < NT:
                    a1_mm_pe(i + 1, (2, 3))
                if i + 2 < NT:
                    a1_pre(i + 2)
                a1_tail2(i)
                if i + 1 < NT:
                    a1_gelu(i + 1)
            P.barrier()
            P.flush()

        mTn = sbt(mid, "mTn", [128, 8, S], BF16)
        with contextlib.ExitStack() as st:
            cqT = sbt(st, "cqT", [128, 4, S], BF16); ckvT = sbt(st, "ckvT", [128, 2, S], BF16); krT = sbt(st, "krT", [64, S], BF16)
            BcqT, BckvT, BkrT = bufs(NT), bufs(NT), bufs(NT)
            with contextlib.ExitStack() as st2:
                w_lat = sbt(st2, "w_lat", [128, 16, 832], BF16); Bwlat = Buf()
                wv = w_in.rearrange("(kc p) n -> p kc n", p=128)
                for hh in range(2):
                    cast_load(w_lat[:, hh * 8:(hh + 1) * 8, :], wv[:, hh * 8:(hh + 1) * 8, 2048:2880], "wlat", Bwlat)
                g1 = sbt(st2, "g1b", [128, 16], F32); Bg1 = Buf()
                gq_t = sbt(st2, "gq_t", [128, 512], F32); gkv_t = sbt(st2, "gkv_t", [128, 256], F32); Bgq, Bgkv = bufs(2)
                P.dma("sp", lambda e: e.dma_start(out=g1[:], in_=g1T[:]), "c0", writes=[Bg1])
                P.dma("sp", lambda e: e.dma_start(out=gq_t[:], in_=gqb[:]), "c1", writes=[Bgq])
                P.dma("sp", lambda e: e.dma_start(out=gkv_t[:], in_=gkvb[:]), "c2", writes=[Bgkv])
                xTt = [sbt(st2, "xTu%d" % k, [128, 16, 128], F32) for k in range(2)]
                xgT = [sbt(st2, "xgU%d" % k, [128, 16, 128], BF16) for k in range(2)]
                cst_k = [sbt(st2, "cstk%d" % k, [128, 2, 64], F32) for k in range(2)]
                BxTt, BxgT, Bcsk = bufs(2), bufs(2), bufs(2)
                lat = [sbt(st2, "lat%d" % k, [128, 832], F32) for k in range(2)]
                latn = [sbt(st2, "latn%d" % k, [128, 832], BF16) for k in range(2)]
                junk = sbt(st2, "junkb", [128, 512], BF16)
                krtmp = [sbt(st2, "krtmp%d" % k, [128, 2, 64], F32) for k in range(2)]
                stt = [sbt(st2, "sttb%d" % k, [128, 8], F32) for k in range(2)]
                Blat, Blatn, Bkrtmp = bufs(2), bufs(2), bufs(2)
                Bjunk = Buf()
                Bst2 = [sbufs(8), sbufs(8)]

                def a2_pre(i):
                    k = i % 2
                    P.dma("sp", lambda e: e.dma_start(out=xTt[k][:], in_=xT[i]), "xTt%d" % k, writes=[BxTt[k]])
                    P.op("dve", lambda e: e.tensor_tensor(out=xgT[k][:], in0=xTt[k][:], in1=g1[:, :].unsqueeze(2).to_broadcast([128, 16, 128]), op=ALU.mult),
                         reads=[BxTt[k], Bg1], writes=[BxgT[k]])

                def a2_cs(i):
                    k = i % 2
                    P.dma("sp", lambda e: e.dma_start(out=cst_k[k][:, 0, :], in_=cs_tok[i * 128:(i + 1) * 128, :]), "csk%d" % k, writes=[Bcsk[k]])
                    P.dma("sp", lambda e: e.dma_start(out=cst_k[k][:, 1, :], in_=sn_tok[i * 128:(i + 1) * 128, :]), "csk%d" % k, writes=[Bcsk[k]])

                def a2_mm(i):
                    k = i % 2
                    b0, b1 = (0, 1) if k == 0 else (4, 5)
                    for kc in range(16):
                        P.op("pe", lambda e, kc=kc: e.matmul(banks[b0][:], xgT[k][:, kc, :], w_lat[:, kc, 0:512], start=(kc == 0), stop=(kc == 15)),
                             reads=[BxgT[k], Bwlat], writes=[PB[b0]], signal=(kc == 15))
                    for kc in range(16):
                        P.op("pe", lambda e, kc=kc: e.matmul(banks[b1][:, 0:320], xgT[k][:, kc, :], w_lat[:, kc, 512:832], start=(kc == 0), stop=(kc == 15)),
                             reads=[BxgT[k], Bwlat], writes=[PB[b1]], signal=(kc == 15))

                def a2_evac(i):
                    k = i % 2
                    b0, b1 = (0, 1) if k == 0 else (4, 5)
                    P.op("act", lambda e: e.activation(out=lat[k][:, 0:512], in_=banks[b0][:], func=AF.Copy, scale=rstd1[:, i:i + 1]), reads=[PB[b0], Brstd1[i]], writes=[Blat[k]])
                    P.op("act", lambda e: e.activation(out=lat[k][:, 512:832], in_=banks[b1][:, 0:320], func=AF.Copy, scale=rstd1[:, i:i + 1]), reads=[PB[b1], Brstd1[i]], writes=[Blat[k]])

                def a2_tail(i):
                    k = i % 2
                    st_, Bst = stt[k], Bst2[k]
                    la, ln, kt = lat[k], latn[k], krtmp[k]
                    P.op("act", lambda e: e.activation(out=junk[:, 0:512], in_=la[:, 0:512], func=AF.Square, accum_out=st_[:, 0:1]), reads=[Blat[k]], writes=[Bjunk, Bst[0]])
                    rstd_from_ss((st_[:, 1:2], Bst[1]), st_[:, 0:1], Bst[0], st_[:, 2:3], Bst[2], 512, EPS)
                    P.op("dve", lambda e: e.scalar_tensor_tensor(out=ln[:, 0:512], in0=la[:, 0:512], scalar=st_[:, 2:3], in1=gq_t[:], op0=ALU.mult, op1=ALU.mult),
                         reads=[Blat[k], Bst[2], Bgq], writes=[Blatn[k]])
                    P.op("act", lambda e: e.activation(out=junk[:, 0:256], in_=la[:, 512:768], func=AF.Square, accum_out=st_[:, 3:4]), reads=[Blat[k]], writes=[Bjunk, Bst[3]])
                    rstd_from_ss((st_[:, 4:5], Bst[4]), st_[:, 3:4], Bst[3], st_[:, 5:6], Bst[5], 256, EPS)
                    P.op("dve", lambda e: e.scalar_tensor_tensor(out=ln[:, 512:768], in0=la[:, 512:768], scalar=st_[:, 5:6], in1=gkv_t[:], op0=ALU.mult, op1=ALU.mult),
                         reads=[Blat[k], Bst[5], Bgkv], writes=[Blatn[k]])
                    P.op("dve", lambda e: e.tensor_tensor(out=kt[:, 0, :], in0=la[:, 768:832], in1=cst_k[k][:, 0, :], op=ALU.mult), reads=[Blat[k], Bcsk[k]], writes=[Bkrtmp[k]])
                    P.op("dve", lambda e: e.tensor_tensor(out=kt[:, 1, 0:32], in0=la[:, 800:832], in1=cst_k[k][:, 1, 0:32], op=ALU.mult), reads=[Blat[k], Bcsk[k]], writes=[Bkrtmp[k]])
                    P.op("dve", lambda e: e.tensor_tensor(out=kt[:, 1, 32:64], in0=la[:, 768:800], in1=cst_k[k][:, 1, 32:64], op=ALU.mult), reads=[Blat[k], Bcsk[k]], writes=[Bkrtmp[k]])
                    P.op("dve", lambda e: e.tensor_tensor(out=ln[:, 768:832], in0=kt[:, 0, :], in1=kt[:, 1, :], op=ALU.add), reads=[Bkrtmp[k]], writes=[Blatn[k]])
                    for c in range(4):
                        P.op("pe", lambda e, c=c: e.transpose(out=bank_bf(2)[:, c * 128:(c + 1) * 128], in_=ln[:, c * 128:(c + 1) * 128], identity=identb[:]),
                             reads=[Blatn[k], Bident], writes=[PB[2]], signal=(c == 3))
                    P.op("act", lambda e: e.activation(out=cqT[:, :, i * 128:(i + 1) * 128], in_=bank_bf(2)[:, 0:512].rearrange("p (c t) -> p c t", c=4), func=AF.Copy),
                         reads=[PB[2]], writes=[BcqT[i]])
                    for c in range(2):
                        P.op("pe", lambda e, c=c: e.transpose(out=bank_bf(3)[:, c * 128:(c + 1) * 128], in_=ln[:, 512 + c * 128:512 + (c + 1) * 128], identity=identb[:]),
                             reads=[Blatn[k], Bident], writes=[PB[3]], signal=False)
                    P.op("pe", lambda e: e.transpose(out=bank_bf(3)[0:64, 256:384], in_=ln[:, 768:832], identity=identb[:]),
                         reads=[Blatn[k], Bident], writes=[PB[3]], signal=True)
                    P.op("dve", lambda e: e.tensor_copy(out=ckvT[:, :, i * 128:(i + 1) * 128], in_=bank_bf(3)[:, 0:256].rearrange("p (c t) -> p c t", c=2)),
                         reads=[PB[3]], writes=[BckvT[i]])
                    P.op("dve", lambda e: e.tensor_copy(out=krT[:, i * 128:(i + 1) * 128], in_=bank_bf(3)[0:64, 256:384]),
                         reads=[PB[3]], writes=[BkrT[i]])

                a2_cs(0)
                a2_cs(1)
                a2_pre(0)
                a2_mm(0)
                a2_evac(0)
                a2_pre(1)
                for i in range(NT):
                    if i + 1 < NT:
                        a2_mm(i + 1)
                    if i + 2 < NT:
                        a2_pre(i + 2)
                    a2_tail(i)
                    if i + 2 < NT:
                        a2_cs(i + 2)
                    if i + 1 < NT:
                        a2_evac(i + 1)
                P.barrier()
                P.flush()
            wuq = sbt(st, "wuq", [128, 4, 8, 256], BF16); wukv = sbt(st, "wukv", [128, 2, 2048], BF16); Bwuq, Bwukv = bufs(2)
            for hh in range(2):
                cast_load(wuq[:, :, hh * 4:(hh + 1) * 4, :], w_uq.rearrange("(kc p) h n -> p kc h n", p=128)[:, :, hh * 4:(hh + 1) * 4, :], "wuq", Bwuq)
                cast_load(wukv[:, :, hh * 1024:(hh + 1) * 1024], w_ukv.rearrange("(kc p) n -> p kc n", p=128)[:, :, hh * 1024:(hh + 1) * 1024], "wukv", Bwukv)
            cs_f = sbt(st, "cs_f", [64, S], F32); sn_f = sbt(st, "sn_f", [64, S], F32); gm_t = sbt(st, "gm_t", [128, 8], F32)
            Bcsf, Bsnf, Bgm = bufs(3)
            P.dma("sp", lambda e: e.dma_start(out=cs_f[:], in_=csT[:]), "c0", writes=[Bcsf])
            P.dma("sp", lambda e: e.dma_start(out=sn_f[:], in_=snT[:]), "c1", writes=[Bsnf])
            P.dma("sp", lambda e: e.dma_start(out=gm_t[:], in_=gm[:]), "c2", writes=[Bgm])
            QnT = [sbt(st, "QnT%d" % k, [128, S], BF16) for k in range(2)]
            QrT = [sbt(st, "QrT%d" % k, [64, S], BF16) for k in range(2)]
            KnT = [sbt(st, "KnT%d" % k, [128, S], BF16) for k in range(2)]
            Vh = [sbt(st, "Vh%d" % k, [128, NT, 128], BF16) for k in range(2)]
            BQn, BQr, BKn, BVh = bufs(2), bufs(2), bufs(2), bufs(2)
            rt1 = sbt(st, "rt1", [64, 512], F32); rt2 = sbt(st, "rt2", [64, 512], F32); Brt1, Brt2 = bufs(2)
            Pt = [sbt(st, "Pt%d" % k, [128, 512], BF16) for k in range(3)]; BPt = bufs(3)
            rec = sbt(st, "rec", [128, 512], F32); Brec = Buf()
            scale = 1.0 / math.sqrt(192.0)
            allT = lambda B_: list(B_)
            pt_i = 0
            s_i = 0
            def b_prep(h):
                k = h % 2
                for G in range(4):
                    gs = slice(G * 512, (G + 1) * 512)
                    tl = [4 * G + t for t in range(4)]
                    for kc in range(4):
                        P.op("pe", lambda e, kc=kc, h=h, gs=gs: e.matmul(banks[0][:], wuq[:, kc, h, 0:128], cqT[:, kc, gs], start=(kc == 0), stop=(kc == 3)),
                             reads=[Bwuq] + [BcqT[t] for t in tl], writes=[PB[0]], signal=(kc == 3))
                    P.op("act", lambda e, k=k, gs=gs: e.activation(out=QnT[k][:, gs], in_=banks[0][:], func=AF.Copy), reads=[PB[0]], writes=[BQn[k]])
                    for kc in range(4):
                        P.op("pe", lambda e, kc=kc, h=h, gs=gs: e.matmul(banks[1][0:64, :], wuq[:, kc, h, 128:192], cqT[:, kc, gs], start=(kc == 0), stop=(kc == 3)),
                             reads=[Bwuq] + [BcqT[t] for t in tl], writes=[PB[1]], signal=(kc == 3))
                    for kc in range(4):
                        P.op("pe", lambda e, kc=kc, h=h, gs=gs: e.matmul(banks[2][0:64, :], wuq[:, kc, h, 192:256], cqT[:, kc, gs], start=(kc == 0), stop=(kc == 3)),
                             reads=[Bwuq] + [BcqT[t] for t in tl], writes=[PB[2]], signal=(kc == 3))
                    P.op("dve", lambda e, gs=gs: e.tensor_tensor(out=rt1[:], in0=banks[1][0:64, :], in1=cs_f[:, gs], op=ALU.mult), reads=[PB[1], Bcsf], writes=[Brt1])
                    P.op("dve", lambda e, gs=gs: e.tensor_tensor(out=rt2[:], in0=banks[2][0:64, :], in1=sn_f[:, gs], op=ALU.mult), reads=[PB[2], Bsnf], writes=[Brt2])
                    P.op("dve", lambda e, k=k, gs=gs: e.tensor_tensor(out=QrT[k][:, gs], in0=rt1[:], in1=rt2[:], op=ALU.add), reads=[Brt1, Brt2], writes=[BQr[k]])
                    for kc in range(2):
                        P.op("pe", lambda e, kc=kc, h=h, gs=gs: e.matmul(banks[3][:], wukv[:, kc, h * 256:h * 256 + 128], ckvT[:, kc, gs], start=(kc == 0), stop=(kc == 1)),
                             reads=[Bwukv] + [BckvT[t] for t in tl], writes=[PB[3]], signal=(kc == 1))
                    P.op("act", lambda e, k=k, gs=gs: e.activation(out=KnT[k][:, gs], in_=banks[3][:], func=AF.Copy), reads=[PB[3]], writes=[BKn[k]])
                    for t in range(4):
                        j = 4 * G + t
                        for kc in range(2):
                            P.op("pe", lambda e, kc=kc, h=h, j=j, t=t: e.matmul(banks[0][:, t * 128:(t + 1) * 128], ckvT[:, kc, j * 128:(j + 1) * 128], wukv[:, kc, h * 256 + 128:h * 256 + 256], start=(kc == 0), stop=(kc == 1)),
                                 reads=[Bwukv, BckvT[j]], writes=[PB[0]], signal=(kc == 1 and t == 3))
                    P.op("dve", lambda e, k=k, G=G: e.tensor_copy(out=Vh[k][:, 4 * G:4 * G + 4, :], in_=banks[0][:].rearrange("p (t d) -> p t d", t=4)), reads=[PB[0]], writes=[BVh[k]])

            cnt = {"s": 0, "p": 0}

            def b_S(h, G, j):
                k = h % 2
                lo = max(0, j * 128 - G * 512)
                q0 = G * 512 + lo
                q1 = (G + 1) * 512
                sb_ = 1 + (cnt["s"] % 3); cnt["s"] += 1
                pi = cnt["p"] % 3; cnt["p"] += 1
                P.op("pe", lambda e: e.matmul(banks[sb_][:, lo:512], KnT[k][:, j * 128:(j + 1) * 128], QnT[k][:, q0:q1], start=True, stop=False),
                     reads=[BKn[k], BQn[k]], writes=[PB[sb_]], signal=False)
                P.op("pe", lambda e: e.matmul(banks[sb_][:, lo:512], krT[:, j * 128:(j + 1) * 128], QrT[k][:, q0:q1], start=False, stop=True),
                     reads=[BkrT[j], BQr[k]], writes=[PB[sb_]], signal=True)
                P.op("act", lambda e: e.activation(out=Pt[pi][:, lo:512], in_=banks[sb_][:, lo:512], func=AF.Exp, scale=scale),
                     reads=[PB[sb_]], writes=[BPt[pi]])
                if j >= 4 * G:
                    P.op("dve", lambda e: e.tensor_tensor(out=Pt[pi][:, lo:lo + 128], in0=Pt[pi][:, lo:lo + 128], in1=maskb[:], op=ALU.mult),
                         reads=[BPt[pi], Bmask], writes=[BPt[pi]])
                return (lo, pi)

            def b_PV(h, G, j, lo, pi):
                k = h % 2
                ob, rb = 4 + (G % 2), 6 + (G % 2)
                nj = 4 * G + 4
                P.op("pe", lambda e: e.matmul(banks[ob][:, lo:512], Vh[k][:, j, :], Pt[pi][:, lo:512], start=(j == 0), stop=(j == nj - 1)),
                     reads=[BVh[k], BPt[pi]], writes=[PB[ob]], signal=False)
                P.op("pe", lambda e: e.matmul(banks[rb][:, lo:512], onesb[:], Pt[pi][:, lo:512], start=(j == 0), stop=(j == nj - 1)),
                     reads=[Bones, BPt[pi]], writes=[PB[rb]], signal=True)
                if j == nj - 1:
                    P.op("dve", lambda e: e.reciprocal(out=rec[:], in_=banks[rb][:]), reads=[PB[rb]], writes=[Brec])
                    P.op("dve", lambda e: e.tensor_tensor(out=mTn[:, h, G * 512:(G + 1) * 512], in0=banks[ob][:], in1=rec[:], op=ALU.mult),
                         reads=[PB[ob], Brec], writes=[BmT[h * 4 + G]])

            b_prep(0)
            for h in range(8):
                if h + 1 < 8:
                    b_prep(h + 1)
                its = [(G, j) for G in range(4) for j in range(4 * G + 4)]
                pend = b_S(h, *its[0])
                for n, (G, j) in enumerate(its):
                    nxt = b_S(h, *its[n + 1]) if n + 1 < len(its) else None
                    b_PV(h, G, j, *pend)
                    pend = nxt
            sq = [sbt(st, "sq%d" % k, [128, 512], BF16) for k in range(2)]; Bsq = bufs(2)
            rbc = sbt(st, "rbc", [128, 512], F32); Brbc = Buf()
            for G in range(4):
                gs = slice(G * 512, (G + 1) * 512)
                for h in range(8):
                    k = h % 2
                    P.op("act", lambda e, k=k, h=h, gs=gs: e.activation(out=sq[k][:], in_=mTn[:, h, gs], func=AF.Square), reads=[BmT[h * 4 + G]], writes=[Bsq[k]])
                    P.op("pe", lambda e, k=k, h=h: e.matmul(banks[0][:], onesb[:], sq[k][:], start=(h == 0), stop=(h == 7)), reads=[Bones, Bsq[k]], writes=[PB[0]], signal=True)
                P.op("dve", lambda e: e.tensor_scalar(out=rbc[:], in0=banks[0][:], scalar1=1.0 / 1024, scalar2=EPS, op0=ALU.mult, op1=ALU.add), reads=[PB[0]], writes=[Brbc])
                P.op("act", lambda e: e.activation(out=rbc[:], in_=rbc[:], func=AF.Sqrt), reads=[Brbc], writes=[Brbc])
                P.op("dve", lambda e: e.reciprocal(out=rbc[:], in_=rbc[:]), reads=[Brbc], writes=[Brbc])
                for h in range(8):
                    P.op("dve", lambda e, h=h, gs=gs: e.scalar_tensor_tensor(out=mTn[:, h, gs], in0=mTn[:, h, gs], scalar=gm_t[:, h:h + 1], in1=rbc[:], op0=ALU.mult, op1=ALU.mult),
                         reads=[Bgm, Brbc], writes=[BmT[h * 4 + G]])
            P.barrier()
            P.flush()

        if debug:
            P.dma("sp", lambda e: e.dma_start(out=dbgM[:, 0:8, :], in_=aT[:]), "dbg", reads=BaT)
            P.dma("sp", lambda e: e.dma_start(out=dbgM[:, 8:16, :], in_=mTn[:]), "dbg", reads=BmT)
        with contextlib.ExitStack() as st:
            wo = sbt(st, "wo", [128, 16, D], BF16); Bwo = bufs(4)
            wov = w_out.rearrange("(kc p) n -> p kc n", p=128)
            for cb in range(4):
                for hh in range(2):
                    cast_load(wo[:, hh * 8:(hh + 1) * 8, cb * 512:(cb + 1) * 512], wov[:, hh * 8:(hh + 1) * 8, cb * 512:(cb + 1) * 512], "wo%d" % cb, Bwo[cb])
            xt = [sbt(st, "xc%d" % k, [128, D], F32) for k in range(2)]; Bxt = bufs(2)
            for i in range(NT):
                k = i % 2
                ts_ = slice(i * 128, (i + 1) * 128)
                P.dma("sp", lambda e, k=k, ts_=ts_: e.dma_start(out=xt[k][:], in_=x[ts_, :]), "xt%d" % k, writes=[Bxt[k]])
                for cb in range(4):
                    bk = (i % 2) * 4 + cb
                    for kc in range(16):
                        rd = [BaT[i]] if kc < 8 else [BmT[(kc - 8) * 4 + i // 4]]
                        P.op("pe", lambda e, kc=kc, cb=cb, bk=bk, ts_=ts_: e.matmul(banks[bk][:], (aT[:, kc, ts_] if kc < 8 else mTn[:, kc - 8, ts_]), wo[:, kc, cb * 512:(cb + 1) * 512], start=(kc == 0), stop=(kc == 15)),
                             reads=rd + [Bwo[cb]], writes=[PB[bk]], signal=(kc == 15))
                    P.op("dve", lambda e, k=k, cb=cb, bk=bk: e.tensor_tensor(out=xt[k][:, cb * 512:(cb + 1) * 512], in0=banks[bk][:], in1=xt[k][:, cb * 512:(cb + 1) * 512], op=ALU.add),
                         reads=[PB[bk], Bxt[k]], writes=[Bxt[k]])
                P.dma("sp", lambda e, k=k, ts_=ts_: e.dma_start(out=X1[ts_, :], in_=xt[k][:]), "x1w%d" % k, reads=[Bxt[k]], writes=[BX1[i]])
            P.barrier()
            P.flush()
        mid.close()
        with contextlib.ExitStack() as st:
            wmq = sbt(st, "wmq", [128, 16, 512], BF16); wmo = sbt(st, "wmo", [128, 4, D], BF16); Bwmq, Bwmo = bufs(2)
            memKT = sbt(st, "memKT", [128, 4, 256], BF16); memV = sbt(st, "memV", [128, 2, 512], BF16); BmemK, BmemV = bufs(2)
            gx_t = sbt(st, "gx_t", [128, D], F32); gmoe_t = sbt(st, "gmoe_t", [128, D], F32); Bgx, Bgmoe = bufs(2)
            wr_t = sbt(st, "wr_t", [128, 16, 72], F32); br_t = sbt(st, "br_t", [128, 72], F32); io_t = sbt(st, "io_t", [128, 64], F32); dm_t = sbt(st, "dm_t", [128, 1], F32)
            Bwr, Bbr, Bio, Bdm = bufs(4)
            for hh in range(2):
                cast_load(wmq[:, hh * 8:(hh + 1) * 8, :], w_mq.rearrange("(kc p) n -> p kc n", p=128)[:, hh * 8:(hh + 1) * 8, :], "wmq", Bwmq)
                cast_load(wmo[:, :, hh * 1024:(hh + 1) * 1024], w_mo.rearrange("(kc p) n -> p kc n", p=128)[:, :, hh * 1024:(hh + 1) * 1024], "wmo", Bwmo)
            P.dma("sp", lambda e: e.dma_start(out=gx_t[:], in_=gxb[:]), "c0", writes=[Bgx])
            P.dma("sp", lambda e: e.dma_start(out=gmoe_t[:], in_=gmoeb[:]), "c1", writes=[Bgmoe])
            P.dma("sp", lambda e: e.dma_start(out=wr_t[:], in_=wr.rearrange("(kc p) n -> p kc n", p=128)), "c2", writes=[Bwr])
            P.dma("sp", lambda e: e.dma_start(out=br_t[:], in_=brb[:]), "c3", writes=[Bbr])
            P.dma("sp", lambda e: e.dma_start(out=io_t[:], in_=iota64[:]), "c4", writes=[Bio])
            P.dma("sp", lambda e: e.dma_start(out=dm_t[:], in_=dummy[:]), "c5", writes=[Bdm])
            xt = [sbt(st, "xd%d" % k, [128, D], F32) for k in range(3)]; Bxt = bufs(3)
            hb = sbt(st, "hb", [128, D], BF16); hT = sbt(st, "hT", [128, 16, 128], BF16); Bhb, BhT = bufs(2)
            junk = sbt(st, "junkc", [128, D], BF16); Bjunk = Buf()
            stt = sbt(st, "sttc", [128, 8], F32); Bst = sbufs(8)
            with contextlib.ExitStack() as st2:
                wmk = sbt(st2, "wmk", [128, 16, 512], BF16); wmv = sbt(st2, "wmv", [128, 16, 512], BF16); Bwmk, Bwmv = bufs(2)
                gmem_t = sbt(st2, "gmem_t", [128, D], F32); Bgmem = Buf()
                memT = sbt(st2, "memT", [128, 16, 256], BF16); BmemT = bufs(2)
                for hh in range(2):
                    cast_load(wmk[:, hh * 8:(hh + 1) * 8, :], w_mk.rearrange("(kc p) n -> p kc n", p=128)[:, hh * 8:(hh + 1) * 8, :], "wmk", Bwmk)
                    cast_load(wmv[:, hh * 8:(hh + 1) * 8, :], w_mv.rearrange("(kc p) n -> p kc n", p=128)[:, hh * 8:(hh + 1) * 8, :], "wmv", Bwmv)
                P.dma("sp", lambda e: e.dma_start(out=gmem_t[:], in_=gmemb[:]), "c6", writes=[Bgmem])
                for mt in range(2):
                    P.dma("sp", lambda e, mt=mt: e.dma_start(out=xt[mt][:], in_=mem[mt * 128:(mt + 1) * 128, :]), "xt%d" % mt, writes=[Bxt[mt]])
                    P.op("act", lambda e, mt=mt: e.activation(out=junk[:], in_=xt[mt][:], func=AF.Square, accum_out=stt[:, 0:1]), reads=[Bxt[mt]], writes=[Bjunk, Bst[0]])
                    rstd_from_ss((stt[:, 1:2], Bst[1]), stt[:, 0:1], Bst[0], stt[:, 2:3], Bst[2], D, EPS)
                    P.op("dve", lambda e, mt=mt: e.scalar_tensor_tensor(out=hb[:], in0=xt[mt][:], scalar=stt[:, 2:3], in1=gmem_t[:], op0=ALU.mult, op1=ALU.mult),
                         reads=[Bxt[mt], Bst[2], Bgmem], writes=[Bhb])
                    for half in range(2):
                        for c in range(8):
                            kc = half * 8 + c
                            P.op("pe", lambda e, c=c, kc=kc, half=half: e.transpose(out=bank_bf(half)[:, c * 128:(c + 1) * 128], in_=hb[:, kc * 128:(kc + 1) * 128], identity=identb[:]),
                                 reads=[Bhb, Bident], writes=[PB[half]], signal=(c == 7))
                        P.op("act", lambda e, half=half, mt=mt: e.activation(out=memT[:, half * 8:(half + 1) * 8, mt * 128:(mt + 1) * 128], in_=bank_bf(half).rearrange("p (c t) -> p c t", c=8), func=AF.Copy),
                             reads=[PB[half]], writes=[BmemT[mt]])
                for h in range(4):
                    for kc in range(16):
                        P.op("pe", lambda e, h=h, kc=kc: e.matmul(banks[2][:, h * 256 % 512:h * 256 % 512 + 256] if h < 2 else banks[3][:, (h - 2) * 256:(h - 2) * 256 + 256], wmk[:, kc, h * 128:(h + 1) * 128], memT[:, kc, :], start=(kc == 0), stop=(kc == 15)),
                             reads=[Bwmk] + BmemT, writes=[PB[2 + h // 2]], signal=(kc == 15))
                for hp in range(2):
                    P.op("act", lambda e, hp=hp: e.activation(out=memKT[:, hp * 2:hp * 2 + 2, :], in_=banks[2 + hp][:].rearrange("p (h m) -> p h m", h=2), func=AF.Copy), reads=[PB[2 + hp]], writes=[BmemK])
                for mt in range(2):
                    for kc in range(16):
                        P.op("pe", lambda e, mt=mt, kc=kc: e.matmul(banks[4 + mt][:], memT[:, kc, mt * 128:(mt + 1) * 128], wmv[:, kc, :], start=(kc == 0), stop=(kc == 15)),
                             reads=[Bwmv] + BmemT, writes=[PB[4 + mt]], signal=(kc == 15))
                    P.op("dve", lambda e, mt=mt: e.tensor_copy(out=memV[:, mt, :], in_=banks[4 + mt][:]), reads=[PB[4 + mt]], writes=[BmemV])
                P.barrier()
                P.flush()
            qmT = [sbt(st, "qmT%d" % k, [128, 4, 128], BF16) for k in range(2)]; Pm = sbt(st, "Pm", [128, 8, 128], BF16); oT = sbt(st, "oT", [128, 4, 128], BF16); rec = sbt(st, "recc", [128, 512], F32)
            BqmT = bufs(2)
            BPm, BoT, Brec = bufs(3)
            h3 = sbt(st, "h3", [128, D], F32); h3T = sbt(st, "h3T", [128, 16, 128], F32); Bh3, Bh3T = bufs(2)
            h3b = [sbt(st, "h3b%d" % k, [128, D], BF16) for k in range(2)]; Bh3b = bufs(2)
            lg = [sbt(st, "lg%d" % k, [128, 72], F32) for k in range(2)]; sm = sbt(st, "sm", [128, 64], F32); Blg = bufs(2); Bsm = sbufs(64)
            E1 = sbt(st, "E1", [128, 64], F32); E2 = sbt(st, "E2", [128, 64], F32); Ef = sbt(st, "Ef", [128, 64], F32); Eb = sbt(st, "Eb", [128, 64], BF16)
            Ecum = sbt(st, "Ecum", [128, 64], BF16); j64 = sbt(st, "j64", [128, 64], F32)
            BE1, BE2, BEf, BEb, BEcum, Bj64 = bufs(6)
            slf = sbt(st, "slf", [128, 2], F32); Bslf = Buf(small=True)
            P.op("dve", lambda e: e.memset(Ecum[:], 0.0), writes=[BEcum])
            sc_m = 1.0 / math.sqrt(128.0)
            def c2_fa(i):
                k = i % 3
                kq = i % 2
                ts_ = slice(i * 128, (i + 1) * 128)
                P.dma("sp", lambda e, k=k, ts_=ts_: e.dma_start(out=xt[k][:], in_=X1[ts_, :]), "xd%d" % k, reads=[BX1[i]], writes=[Bxt[k]])
                P.op("act", lambda e, k=k: e.activation(out=junk[:], in_=xt[k][:], func=AF.Square, accum_out=stt[:, 0:1]), reads=[Bxt[k]], writes=[Bjunk, Bst[0]])
                rstd_from_ss((stt[:, 1:2], Bst[1]), stt[:, 0:1], Bst[0], stt[:, 2:3], Bst[2], D, EPS)
                P.op("dve", lambda e, k=k: e.scalar_tensor_tensor(out=hb[:], in0=xt[k][:], scalar=stt[:, 2:3], in1=gx_t[:], op0=ALU.mult, op1=ALU.mult),
                     reads=[Bxt[k], Bst[2], Bgx], writes=[Bhb])
                yield
                for half in range(2):
                    for c in range(8):
                        kc = half * 8 + c
                        P.op("pe", lambda e, c=c, kc=kc, half=half: e.transpose(out=bank_bf(half)[:, c * 128:(c + 1) * 128], in_=hb[:, kc * 128:(kc + 1) * 128], identity=identb[:]),
                             reads=[Bhb, Bident], writes=[PB[half]], signal=(c == 7))
                    P.op("act" if half == 0 else "dve", (lambda e, half=half: e.activation(out=hT[:, half * 8:(half + 1) * 8, :], in_=bank_bf(half).rearrange("p (c t) -> p c t", c=8), func=AF.Copy)) if half == 0 else
                         (lambda e, half=half: e.tensor_copy(out=hT[:, half * 8:(half + 1) * 8, :], in_=bank_bf(half).rearrange("p (c t) -> p c t", c=8))),
                         reads=[PB[half]], writes=[BhT])
                yield
                for h in range(4):
                    for kc in range(16):
                        P.op("pe", lambda e, h=h, kc=kc: e.matmul(banks[2][:, h * 128:(h + 1) * 128], wmq[:, kc, h * 128:(h + 1) * 128], hT[:, kc, :], start=(kc == 0), stop=(kc == 15)),
                             reads=[Bwmq, BhT], writes=[PB[2]], signal=(kc == 15 and h == 3))
                P.op("act", lambda e: e.activation(out=qmT[kq][:], in_=banks[2][:].rearrange("p (h t) -> p h t", h=4), func=AF.Copy), reads=[PB[2]], writes=[BqmT[kq]])
                yield

            def c2_fb(i):
                k = i % 3
                kq = i % 2
                ts_ = slice(i * 128, (i + 1) * 128)
                for h in range(4):
                    for mt in range(2):
                        c = h * 2 + mt
                        bk = 3 + c // 4
                        P.op("pe", lambda e, h=h, mt=mt, c=c, bk=bk: e.matmul(banks[bk][:, (c % 4) * 128:(c % 4 + 1) * 128], memKT[:, h, mt * 128:(mt + 1) * 128], qmT[kq][:, h, :], start=True, stop=True),
                             reads=[BmemK, BqmT[kq]], writes=[PB[bk]], signal=(c % 4 == 3))
                for hp in range(2):
                    P.op("act", lambda e, hp=hp: e.activation(out=Pm[:, hp * 4:hp * 4 + 4, :], in_=banks[3 + hp][:].rearrange("p (c t) -> p c t", c=4), func=AF.Exp, scale=sc_m),
                         reads=[PB[3 + hp]], writes=[BPm])
                yield
                for h in range(4):
                    for mt in range(2):
                        P.op("pe", lambda e, h=h, mt=mt: e.matmul(banks[3][:, h * 128:(h + 1) * 128], memV[:, mt, h * 128:(h + 1) * 128], Pm[:, h * 2 + mt, :], start=(mt == 0), stop=(mt == 1)),
                             reads=[BmemV, BPm], writes=[PB[3]], signal=False)
                    for mt in range(2):
                        P.op("pe", lambda e, h=h, mt=mt: e.matmul(banks[4][:, h * 128:(h + 1) * 128], onesb[:], Pm[:, h * 2 + mt, :], start=(mt == 0), stop=(mt == 1)),
                             reads=[Bones, BPm], writes=[PB[3], PB[4]], signal=(mt == 1 and h == 3))
                P.op("dve", lambda e: e.reciprocal(out=rec[:], in_=banks[4][:]), reads=[PB[4]], writes=[Brec])
                P.op("dve", lambda e: e.tensor_tensor(out=oT[:].rearrange("p h t -> p (h t)"), in0=banks[3][:], in1=rec[:], op=ALU.mult), reads=[PB[3], Brec], writes=[BoT])
                yield
                for cb in range(4):
                    bk = 3 if cb % 2 == 0 else 4
                    for h in range(4):
                        P.op("pe", lambda e, h=h, cb=cb, bk=bk: e.matmul(banks[bk][:], oT[:, h, :], wmo[:, h, cb * 512:(cb + 1) * 512], start=(h == 0), stop=(h == 3)),
                             reads=[BoT, Bwmo], writes=[PB[bk]], signal=(h == 3))
                    P.op("dve", lambda e, k=k, cb=cb, bk=bk: e.tensor_tensor(out=xt[k][:, cb * 512:(cb + 1) * 512], in0=banks[bk][:], in1=xt[k][:, cb * 512:(cb + 1) * 512], op=ALU.add),
                         reads=[PB[bk], Bxt[k]], writes=[Bxt[k]])
                yield
                P.dma("sp", lambda e, k=k, ts_=ts_: e.dma_start(out=X2[ts_, :], in_=xt[k][:]), "x2w%d" % k, reads=[Bxt[k]], writes=[BX2[i]])
                yield

            def c2_ba(i):
                k = i % 3
                kh = i % 2
                ts_ = slice(i * 128, (i + 1) * 128)
                P.op("act", lambda e, k=k: e.activation(out=junk[:], in_=xt[k][:], func=AF.Square, accum_out=stt[:, 3:4]), reads=[Bxt[k]], writes=[Bjunk, Bst[3]])
                rstd_from_ss((stt[:, 4:5], Bst[4]), stt[:, 3:4], Bst[3], stt[:, 5:6], Bst[5], D, EPS)
                P.op("dve", lambda e, k=k: e.scalar_tensor_tensor(out=h3[:], in0=xt[k][:], scalar=stt[:, 5:6], in1=gmoe_t[:], op0=ALU.mult, op1=ALU.mult),
                     reads=[Bxt[k], Bst[5], Bgmoe], writes=[Bh3])
                P.op("act", lambda e, k=k: e.activation(out=h3b[kh][:], in_=h3[:], func=AF.Copy), reads=[Bh3], writes=[Bh3b[kh]])
                yield
                for q4 in range(4):
                    for c in range(4):
                        kc = q4 * 4 + c
                        P.op("pe", lambda e, c=c, kc=kc, q4=q4: e.transpose(out=banks[5 + q4 % 2][:, c * 128:(c + 1) * 128], in_=h3[:, kc * 128:(kc + 1) * 128], identity=identf),
                             reads=[Bh3, Bcst], writes=[PB[5 + q4 % 2]], signal=(c == 3))
                    P.op("act" if q4 % 2 == 0 else "dve", (lambda e, q4=q4: e.activation(out=h3T[:, q4 * 4:q4 * 4 + 4, :], in_=banks[5 + q4 % 2][:].rearrange("p (c t) -> p c t", c=4), func=AF.Copy)) if q4 % 2 == 0 else
                         (lambda e, q4=q4: e.tensor_copy(out=h3T[:, q4 * 4:q4 * 4 + 4, :], in_=banks[5 + q4 % 2][:].rearrange("p (c t) -> p c t", c=4))),
                         reads=[PB[5 + q4 % 2]], writes=[Bh3T])
                yield
                for kc in range(16):
                    P.op("pe", lambda e, kc=kc: e.matmul(banks[7][:, 0:72], h3T[:, kc, :], wr_t[:, kc, :], start=(kc == 0), stop=(kc == 15)), reads=[Bh3T, Bwr], writes=[PB[7]], signal=(kc == 15))
                P.op("dve", lambda e: e.tensor_tensor(out=lg[kh][:], in0=banks[7][:, 0:72], in1=br_t[:], op=ALU.add), reads=[PB[7], Bbr], writes=[Blg[kh]])
                yield

            def c2_bb(i):
                k = i % 3
                kh = i % 2
                ts_ = slice(i * 128, (i + 1) * 128)
                Bs = Bsm
                V = lambda a, b: sm[:, a:b]
                P.op("dve", lambda e: e.max(out=V(0, 8), in_=lg[kh][:, 0:8]), reads=[Blg[kh]], writes=[Bs[0]])
                P.op("dve", lambda e: e.tensor_scalar(out=V(8, 9), in0=V(0, 1), scalar1=-1.0, scalar2=None, op0=ALU.mult), reads=[Bs[0]], writes=[Bs[8]])
                P.op("act", lambda e: e.activation(out=V(16, 24), in_=lg[kh][:, 0:8], func=AF.Exp, bias=V(8, 9), accum_out=V(9, 10)), reads=[Blg[kh], Bs[8]], writes=[Bs[16], Bs[9]])
                P.op("dve", lambda e: e.tensor_scalar(out=V(24, 32), in0=lg[kh][:, 0:8], scalar1=V(0, 1), scalar2=None, op0=ALU.is_equal), reads=[Blg[kh], Bs[0]], writes=[Bs[24]])
                yield
                P.op("dve", lambda e: e.tensor_scalar(out=V(32, 40), in0=lg[kh][:, 8:16], scalar1=V(24, 25), scalar2=None, op0=ALU.mult), reads=[Blg[kh], Bs[24]], writes=[Bs[32]])
                for g in range(1, 8):
                    P.op("dve", lambda e, g=g: e.scalar_tensor_tensor(out=V(32, 40), in0=lg[kh][:, 8 + g * 8:16 + g * 8], scalar=V(24 + g, 25 + g), in1=V(32, 40), op0=ALU.mult, op1=ALU.add),
                         reads=[Blg[kh], Bs[24], Bs[32]], writes=[Bs[32]])
                P.op("dve", lambda e: e.max(out=V(40, 48), in_=V(32, 40)), reads=[Bs[32]], writes=[Bs[40]])
                yield
                P.op("dve", lambda e: e.tensor_tensor(out=V(10, 11), in0=V(41, 42), in1=V(40, 41), op=ALU.subtract), reads=[Bs[40]], writes=[Bs[10]])
                P.op("act", lambda e: e.activation(out=V(11, 12), in_=V(10, 11), func=AF.Exp), reads=[Bs[10]], writes=[Bs[11]])
                P.op("dve", lambda e: e.scalar_tensor_tensor(out=V(12, 13), in0=V(11, 12), scalar=1.0, in1=V(9, 10), op0=ALU.add, op1=ALU.mult), reads=[Bs[11], Bs[9]], writes=[Bs[12]])
                P.op("dve", lambda e: e.reciprocal(out=V(13, 14), in_=V(12, 13)), reads=[Bs[12]], writes=[Bs[13]])
                P.op("dve", lambda e: e.tensor_tensor(out=V(14, 15), in0=V(11, 12), in1=V(13, 14), op=ALU.mult), reads=[Bs[11], Bs[13]], writes=[Bs[14]])
                yield
                P.op("dve", lambda e: e.tensor_scalar(out=V(48, 56), in0=V(32, 40), scalar1=V(40, 41), scalar2=None, op0=ALU.is_equal), reads=[Bs[32], Bs[40]], writes=[Bs[48]])
                P.op("dve", lambda e: e.tensor_scalar(out=V(56, 64), in0=V(32, 40), scalar1=V(41, 42), scalar2=None, op0=ALU.is_equal), reads=[Bs[32], Bs[40]], writes=[Bs[56]])
                ohg_b = V(24, 32).unsqueeze(2).to_broadcast([128, 8, 8])
                P.op("dve", lambda e: e.tensor_tensor(out=E1[:].rearrange("p (g j) -> p g j", g=8), in0=ohg_b, in1=V(48, 56).unsqueeze(1).to_broadcast([128, 8, 8]), op=ALU.mult),
                     reads=[Bs[24], Bs[48]], writes=[BE1])
                P.op("dve", lambda e: e.tensor_tensor(out=E2[:].rearrange("p (g j) -> p g j", g=8), in0=ohg_b, in1=V(56, 64).unsqueeze(1).to_broadcast([128, 8, 8]), op=ALU.mult),
                     reads=[Bs[24], Bs[56]], writes=[BE2])
                P.op("dve", lambda e: e.tensor_tensor(out=Eb[:], in0=E1[:], in1=E2[:], op=ALU.add), reads=[BE1, BE2], writes=[BEb])
                yield
                P.op("pe", lambda e, i=i: e.matmul(banks[7][:, 128:192], ltrib[:], Eb[:], start=True, stop=(i == 0)), reads=[Bltri, BEb], writes=[PB[7]], signal=(i == 0))
                if i > 0:
                    P.op("pe", lambda e: e.matmul(banks[7][:, 128:192], onesb[:], Ecum[:], start=False, stop=True), reads=[Bones, BEcum], writes=[PB[7]], signal=True)
                P.op("dve", lambda e: e.tensor_tensor(out=Ecum[:], in0=Ecum[:], in1=Eb[:], op=ALU.add), reads=[BEb, BEcum], writes=[BEcum])
                yield
                for kk, (Ek, BEk) in enumerate(((E1, BE1), (E2, BE2))):
                    c0 = 16 + kk * 4
                    P.op("dve", lambda e, Ek=Ek, c0=c0: e.scalar_tensor_tensor(out=j64[:], in0=Ek[:], scalar=1.0, in1=banks[7][:, 128:192], op0=ALU.mult, op1=ALU.mult, accum_out=V(c0, c0 + 1)),
                         reads=[BEk, PB[7]], writes=[Bj64, Bs[c0]])
                    P.op("dve", lambda e, Ek=Ek, c0=c0: e.scalar_tensor_tensor(out=j64[:], in0=Ek[:], scalar=1.0, in1=io_t[:], op0=ALU.mult, op1=ALU.mult, accum_out=V(c0 + 1, c0 + 2)),
                         reads=[BEk, Bio], writes=[Bj64, Bs[c0 + 1]])
                    P.op("dve", lambda e, c0=c0: e.scalar_tensor_tensor(out=V(c0 + 2, c0 + 3), in0=V(c0 + 1, c0 + 2), scalar=float(CAP), in1=V(c0, c0 + 1), op0=ALU.mult, op1=ALU.add),
                         reads=[Bs[c0], Bs[c0 + 1]], writes=[Bs[c0 + 2]])
                    P.op("dve", lambda e, c0=c0: e.tensor_scalar(out=V(c0 + 3, c0 + 4), in0=V(c0, c0 + 1), scalar1=CAP - 0.5, scalar2=None, op0=ALU.is_lt), reads=[Bs[c0]], writes=[Bs[c0 + 3]])
                    P.op("dve", lambda e, c0=c0: e.scalar_tensor_tensor(out=V(c0 + 2, c0 + 3), in0=V(c0 + 2, c0 + 3), scalar=dm_t[:, 0:1], in1=V(c0 + 3, c0 + 4), op0=ALU.subtract, op1=ALU.mult),
                         reads=[Bs[c0 + 2], Bs[c0 + 3], Bdm], writes=[Bs[c0 + 2]])
                    P.op("dve", lambda e, c0=c0, kk=kk: e.tensor_tensor(out=slf[:, kk:kk + 1], in0=V(c0 + 2, c0 + 3), in1=dm_t[:, 0:1], op=ALU.add), reads=[Bs[c0 + 2], Bdm], writes=[Bslf])
                    P.op("dve", lambda e, c0=c0, kk=kk, i=i: e.tensor_tensor(out=gates[:, i, kk:kk + 1], in0=V(13 + kk, 14 + kk), in1=V(c0 + 3, c0 + 4), op=ALU.mult),
                         reads=[Bs[13 + kk], Bs[c0 + 3]], writes=[Bslots[i]])
                P.op("dve", lambda e, i=i: e.tensor_copy(out=slots[:, i, :], in_=slf[:]), reads=[Bslf], writes=[Bslots[i]])
                for kk in range(2):
                    P.dma("pool", lambda e, k=k, i=i, kk=kk: e.indirect_dma_start(out=Xbuf[:, :], out_offset=bass.IndirectOffsetOnAxis(ap=slots[:, i, kk:kk + 1], axis=0), in_=h3b[kh][:], in_offset=None),
                          "scat%d" % kh, reads=[Bh3b[kh], Bslots[i]], writes=[BXb])
                yield

            def interleave(gens):
                active = [g for g in gens if g is not None]
                while active:
                    for g in list(active):
                        try:
                            next(g)
                        except StopIteration:
                            active.remove(g)

            for s in range(-2, NT + 1):
                interleave([c2_fa(s + 2) if 0 <= s + 2 < NT else None,
                            c2_fb(s + 1) if 0 <= s + 1 < NT else None,
                            c2_ba(s) if 0 <= s < NT else None,
                            c2_bb(s - 1) if 0 <= s - 1 < NT else None])
            if debug:
                P.dma("sp", lambda e: e.dma_start(out=dbgS[:, :, 0:2], in_=gates[:]), "dbg", reads=Bslots)
            P.barrier()
            P.flush()

        with contextlib.ExitStack() as st:
            Wg = [sbt(st, "Wg%d" % k, [128, 16, 512], BF16) for k in range(2)]
            Wu = [sbt(st, "Wu%d" % k, [128, 16, 512], BF16) for k in range(2)]
            Wd = [sbt(st, "Wd%d" % k, [128, 4, D], BF16) for k in range(2)]
            BWg, BWu, BWd = bufs(2), bufs(2), bufs(2)
            Xe = [sbt(st, "Xe%d" % k, [128, D], BF16) for k in range(2)]; BXe = bufs(2)
            XT = sbt(st, "XT", [128, 16, 128], BF16); BXT = Buf()
            sg = sbt(st, "sg", [128, 512], F32); Hh = sbt(st, "Hh", [128, 512], BF16); HT = sbt(st, "HT", [128, 4, 128], BF16); Bsg, BHh, BHT = bufs(3)
            Ys = [sbt(st, "Ys%d" % k, [128, D], BF16) for k in range(2)]; BYs = bufs(2)
            zt = sbt(st, "zt", [128, D], BF16); Bzt = Buf()
            P.op("dve", lambda e: e.memset(zt[:], 0.0), writes=[Bzt])
            P.dma("sp", lambda e: e.dma_start(out=Ybuf[NEXP * CAP:NSLOT, :], in_=zt[:]), "yz", reads=[Bzt], writes=[BYb])

            def load_expert(ex):
                k = ex % 2
                P.dma("pool", lambda e, k=k, ex=ex: e.dma_start(out=Wg[k][:].rearrange("p kc n -> p (kc n)"), in_=w_g[ex], max_dma_last_dim=8192), "wg%d" % k, writes=[BWg[k]])
                P.dma("pool", lambda e, k=k, ex=ex: e.dma_start(out=Wu[k][:].rearrange("p kc n -> p (kc n)"), in_=w_u[ex], max_dma_last_dim=8192), "wu%d" % k, writes=[BWu[k]])
                P.dma("pool", lambda e, k=k, ex=ex: e.dma_start(out=Wd[k][:].rearrange("p kc n -> p (kc n)"), in_=w_d[ex], max_dma_last_dim=8192), "wd%d" % k, writes=[BWd[k]])

            def load_x(gb):
                kx = gb % 2
                P.dma("sp", lambda e, kx=kx, gb=gb: e.dma_start(out=Xe[kx][:], in_=Xbuf[gb * 128:(gb + 1) * 128, :]), "xe%d" % kx, reads=[BXb], writes=[BXe[kx]])

            def stage_T(gb):
                kx = gb % 2
                for half in range(2):
                    for c in range(8):
                        kc = half * 8 + c
                        P.op("pe", lambda e, c=c, kc=kc, half=half, kx=kx: e.transpose(out=bank_bf(half)[:, c * 128:(c + 1) * 128], in_=Xe[kx][:, kc * 128:(kc + 1) * 128], identity=identb[:]),
                             reads=[BXe[kx], Bident], writes=[PB[half]], signal=(c == 7))
                    P.op("act" if half == 0 else "dve", (lambda e, half=half: e.activation(out=XT[:, half * 8:(half + 1) * 8, :], in_=bank_bf(half).rearrange("p (c t) -> p c t", c=8), func=AF.Copy)) if half == 0 else
                         (lambda e, half=half: e.tensor_copy(out=XT[:, half * 8:(half + 1) * 8, :], in_=bank_bf(half).rearrange("p (c t) -> p c t", c=8))),
                         reads=[PB[half]], writes=[BXT])

            def stage_GU(gb):
                k = (gb // NBLK) % 2
                for kc in range(16):
                    P.op("pe", lambda e, kc=kc, k=k: e.matmul(banks[2][:], XT[:, kc, :], Wg[k][:, kc, :], start=(kc == 0), stop=(kc == 15)), reads=[BXT, BWg[k]], writes=[PB[2]], signal=(kc == 15))
                for kc in range(16):
                    P.op("pe", lambda e, kc=kc, k=k: e.matmul(banks[3][:], XT[:, kc, :], Wu[k][:, kc, :], start=(kc == 0), stop=(kc == 15)), reads=[BXT, BWu[k]], writes=[PB[3]], signal=(kc == 15))
                P.op("act", lambda e: e.activation(out=sg[:], in_=banks[2][:], func=AF.Silu), reads=[PB[2]], writes=[Bsg])
                P.op("dve", lambda e: e.tensor_tensor(out=Hh[:], in0=banks[3][:], in1=sg[:], op=ALU.mult), reads=[PB[3], Bsg], writes=[BHh])

            def stage_D(gb):
                k = (gb // NBLK) % 2
                kx = gb % 2
                for c in range(4):
                    P.op("pe", lambda e, c=c: e.transpose(out=bank_bf(4)[:, c * 128:(c + 1) * 128], in_=Hh[:, c * 128:(c + 1) * 128], identity=identb[:]), reads=[BHh, Bident], writes=[PB[4]], signal=(c == 3))
                P.op("dve", lambda e: e.tensor_copy(out=HT[:], in_=bank_bf(4)[:, 0:512].rearrange("p (c t) -> p c t", c=4)), reads=[PB[4]], writes=[BHT])
                for cb in range(4):
                    bk = 5 + cb % 3
                    for c in range(4):
                        P.op("pe", lambda e, c=c, cb=cb, bk=bk, k=k: e.matmul(banks[bk][:], HT[:, c, :], Wd[k][:, c, cb * 512:(cb + 1) * 512], start=(c == 0), stop=(c == 3)),
                             reads=[BHT, BWd[k]], writes=[PB[bk]], signal=(c == 3))
                    if cb % 2 == 0:
                        P.op("act", lambda e, cb=cb, bk=bk, kx=kx: e.activation(out=Ys[kx][:, cb * 512:(cb + 1) * 512], in_=banks[bk][:], func=AF.Copy), reads=[PB[bk]], writes=[BYs[kx]])
                    else:
                        P.op("dve", lambda e, cb=cb, bk=bk, kx=kx: e.tensor_copy(out=Ys[kx][:, cb * 512:(cb + 1) * 512], in_=banks[bk][:]), reads=[PB[bk]], writes=[BYs[kx]])
                P.dma("sp", lambda e, kx=kx, gb=gb: e.dma_start(out=Ybuf[gb * 128:(gb + 1) * 128, :], in_=Ys[kx][:]), "yw%d" % kx, reads=[BYs[kx]], writes=[BYb])

            NGB = NEXP * NBLK
            load_expert(0)
            load_expert(1)
            load_x(0)
            load_x(1)
            stage_T(0)
            for gb in range(NGB):
                stage_GU(gb)
                if gb + 1 < NGB:
                    stage_T(gb + 1)
                stage_D(gb)
                if gb + 2 < NGB:
                    load_x(gb + 2)
                if gb % NBLK == NBLK - 1 and gb // NBLK + 2 < NEXP:
                    load_expert(gb // NBLK + 2)
            P.barrier()
            P.flush()

        with contextlib.ExitStack() as st:
            NX, NY = 3, 4
            gf_t = sbt(st, "gf_t", [128, D], F32); Bgf = Buf()
            P.dma("sp", lambda e: e.dma_start(out=gf_t[:], in_=gfb[:]), "c0", writes=[Bgf])
            xt = [sbt(st, "xe_%d" % k, [128, D], F32) for k in range(NX)]; Bxt = bufs(NX)
            y0 = [sbt(st, "y0_%d" % k, [128, D], BF16) for k in range(NY)]; y1 = [sbt(st, "y1_%d" % k, [128, D], BF16) for k in range(NY)]; By0, By1 = bufs(NY), bufs(NY)
            ot = [sbt(st, "ot_%d" % k, [128, D], F32) for k in range(NX)]; Bot = bufs(NX)
            junk = sbt(st, "junke", [128, D], BF16); Bjunk = Buf()
            stt = [sbt(st, "stte%d" % k, [128, 4], F32) for k in range(2)]; Bst2 = [sbufs(4), sbufs(4)]

            def e_load(i):
                kx, ky = i % NX, i % NY
                ts_ = slice(i * 128, (i + 1) * 128)
                P.dma("sp", lambda e: e.dma_start(out=xt[kx][:], in_=X2[ts_, :]), "xt%d" % kx, reads=[BX2[i]], writes=[Bxt[kx]])
                P.dma("pool", lambda e: e.indirect_dma_start(out=y0[ky][:], out_offset=None, in_=Ybuf[:, :], in_offset=bass.IndirectOffsetOnAxis(ap=slots[:, i, 0:1], axis=0)),
                      "g0_%d" % ky, reads=[BYb, Bslots[i]], writes=[By0[ky]])
                P.dma("pool", lambda e: e.indirect_dma_start(out=y1[ky][:], out_offset=None, in_=Ybuf[:, :], in_offset=bass.IndirectOffsetOnAxis(ap=slots[:, i, 1:2], axis=0)),
                      "g1_%d" % ky, reads=[BYb, Bslots[i]], writes=[By1[ky]])

            def e_comp(i):
                kx, ky = i % NX, i % NY
                st_, Bst = stt[i % 2], Bst2[i % 2]
                ts_ = slice(i * 128, (i + 1) * 128)
                P.op("dve", lambda e: e.scalar_tensor_tensor(out=xt[kx][:], in0=y0[ky][:], scalar=gates[:, i, 0:1], in1=xt[kx][:], op0=ALU.mult, op1=ALU.add),
                     reads=[By0[ky], Bslots[i], Bxt[kx]], writes=[Bxt[kx]])
                P.op("dve", lambda e: e.scalar_tensor_tensor(out=xt[kx][:], in0=y1[ky][:], scalar=gates[:, i, 1:2], in1=xt[kx][:], op0=ALU.mult, op1=ALU.add),
                     reads=[By1[ky], Bslots[i], Bxt[kx]], writes=[Bxt[kx]])
                P.op("act", lambda e: e.activation(out=junk[:], in_=xt[kx][:], func=AF.Square, accum_out=st_[:, 0:1]), reads=[Bxt[kx]], writes=[Bjunk, Bst[0]])
                rstd_from_ss((st_[:, 1:2], Bst[1]), st_[:, 0:1], Bst[0], st_[:, 2:3], Bst[2], D, EPS)
                P.op("dve", lambda e: e.scalar_tensor_tensor(out=ot[kx][:], in0=xt[kx][:], scalar=st_[:, 2:3], in1=gf_t[:], op0=ALU.mult, op1=ALU.mult),
                     reads=[Bxt[kx], Bst[2], Bgf], writes=[Bot[kx]])
                P.dma("sp", lambda e: e.dma_start(out=y[ts_, :], in_=ot[kx][:]), "yo%d" % kx, reads=[Bot[kx]])

            for i in range(min(NX, NT)):
                e_load(i)
            for i in range(NT):
                e_comp(i)
                if i + NX < NT:
                    e_load(i + NX)
            P.barrier()
            P.flush()
    return nc


def _bcast(v):
    return np.ascontiguousarray(np.broadcast_to(np.asarray(v, np.float32)[None, :], (128, v.shape[0])))


def prep_shared(inp):
    f = lambda a: np.ascontiguousarray(np.asarray(a, dtype=np.float32))
    sh = {}
    sh["g1T"] = f(inp["g_norm_mix"].reshape(16, 128).T)
    sh["w_in"] = f(inp["w_in"])
    sh["gvb"] = _bcast(inp["g_v"]); sh["bvb"] = _bcast(inp["b_v"]); sh["gab"] = _bcast(inp["g_out_gmlp"])
    sh["wsT"] = f(np.transpose(inp["w_spatial"], (2, 0, 1)))
    sh["bspT"] = f(inp["b_spatial"].T)
    sh["gqb"] = _bcast(inp["g_q_lora"]); sh["gkvb"] = _bcast(inp["g_kv_lora"])
    wq = np.asarray(inp["w_uq"], np.float32).reshape(512, 8, 192)
    sh["w_uq"] = f(np.concatenate([wq[:, :, 0:128], wq[:, :, 128:192], wq[:, :, 160:192], wq[:, :, 128:160]], axis=2))
    sh["w_ukv"] = f(inp["w_ukv"])
    sh["gm"] = f(inp["g_out_mla"].reshape(8, 128).T)
    sh["w_out"] = f(inp["w_out"])
    sh["gxb"] = _bcast(inp["g_norm_xattn"]); sh["gmemb"] = _bcast(inp["g_norm_mem"]); sh["gmoeb"] = _bcast(inp["g_norm_moe"]); sh["gfb"] = _bcast(inp["g_final"])
    sh["w_mq"] = f(inp["w_mq"]); sh["w_mk"] = f(inp["w_mk"]); sh["w_mv"] = f(inp["w_mv"]); sh["w_mo"] = f(inp["w_mo"])
    sh["wr"] = f(np.concatenate([inp["w_router_group"], inp["w_router_expert"]], axis=1))
    sh["brb"] = _bcast(np.concatenate([inp["b_router_group"], inp["b_router_expert"]]))
    def lay(w, kc):
        w = np.asarray(w, np.float32)
        e_, r_, n_ = w.shape
        return np.ascontiguousarray(w.reshape(e_, kc, 128, n_).transpose(0, 2, 1, 3)).reshape(e_, 128, kc * n_)
    sh["w_g"] = lay(inp["w_exp_gate"], 16); sh["w_u"] = lay(inp["w_exp_up"], 16); sh["w_d"] = lay(inp["w_exp_down"], 4)
    pos = np.arange(S, dtype=np.float32)
    inv = (np.float32(10000.0) ** (-np.arange(0, 64, 2, dtype=np.float32) / np.float32(64))).astype(np.float32)
    ang = (pos[:, None] * inv[None, :]).astype(np.float32)
    c, s_ = np.cos(ang).astype(np.float32), np.sin(ang).astype(np.float32)
    sh["cs_tok"] = f(np.concatenate([c, c], axis=1)); sh["sn_tok"] = f(np.concatenate([-s_, s_], axis=1))
    sh["csT"] = f(sh["cs_tok"].T); sh["snT"] = f(sh["sn_tok"].T)
    ii = np.arange(128)
    consts = np.zeros((128, 5, 128), np.float32)
    consts[:, 0, :] = np.eye(128)
    consts[:, 1, :] = (ii[:, None] <= ii[None, :])
    consts[:, 2, :] = (ii[:, None] < ii[None, :])
    consts[:, 3, :] = 1.0
    sh["consts"] = consts
    sh["iota64"] = f(np.broadcast_to(np.arange(64, dtype=np.float32)[None, :], (128, 64)))
    sh["dummyrow"] = f((NEXP * CAP + ii).reshape(128, 1))
    return sh


def prep_core(inp, b):
    xb = np.asarray(inp["x"][b], np.float32)
    return {
        "x": np.ascontiguousarray(xb),
        "xT": np.ascontiguousarray(xb.reshape(NT, 128, 16, 128).transpose(0, 3, 2, 1)),
        "mem": np.ascontiguousarray(np.asarray(inp["mem"][b], np.float32)),
    }


_NC_CACHE = {}


def kernel(**inputs):
    if "nc" not in _NC_CACHE:
        _NC_CACHE["nc"] = build_program(False)
    nc = _NC_CACHE["nc"]
    sh = prep_shared(inputs)
    in_maps = []
    for b in range(8):
        m = dict(sh)
        m.update(prep_core(inputs, b))
        in_maps.append(m)
    res = run_bass_kernel_spmd(nc, in_maps, core_ids=list(range(8)))
    return np.stack([np.asarray(r["y"], np.float32) for r in res.results], axis=0)
```

```python
import contextlib
import math
import numpy as np
import concourse.bass as bass
import concourse.mybir as mybir
from concourse.bass_utils import run_bass_kernel_spmd

F32 = mybir.dt.float32
BF16 = mybir.dt.bfloat16
I32 = mybir.dt.int32
AF = mybir.ActivationFunctionType
ALU = mybir.AluOpType

D = 2048
S = 2048
NT = 16
NEXP = 64
CAP = 256
NBLK = CAP // 128
NSLOT = NEXP * CAP + 128
EPS = 1e-6
ENGS = ("pe", "act", "dve", "pool", "sp")
BLK = {"pe": "tensor", "act": "scalar", "dve": "vector", "pool": "gpsimd", "sp": "sync"}


class Buf:
    __slots__ = ("name", "w", "r", "small")

    def __init__(self, name="", small=False):
        self.name = name
        self.w = None
        self.r = []
        self.small = small


def bufs(n):
    return [Buf() for _ in range(n)]


def sbufs(n):
    return [Buf(small=True) for _ in range(n)]


class Prog:
    NOSYNC_SAME = ("pe", "sp")

    def __init__(self, nc, block, stack):
        self.nc = nc
        self.block = block
        self.eng = {"pe": nc.tensor, "act": nc.scalar, "dve": nc.vector, "pool": nc.gpsimd, "sp": nc.sync}
        self.sem = {e: stack.enter_context(nc.semaphore("s_" + e)) for e in ENGS}
        self.cnt = {e: 0 for e in ENGS}
        self.q = {e: [] for e in ENGS}
        self.seen = {e: {} for e in ENGS}
        self.dsem = {}
        self.stack = stack

    def _semh(self, key):
        return self.sem[key] if key in self.sem else self.dsem[key][0]

    def _deps(self, e, reads, writes):
        need = {}

        def add(dep, small):
            if dep is None:
                return
            k, v = dep
            if k == e and (e in ("pe", "sp") or (e in self.NOSYNC_SAME and not small)):
                return
            if need.get(k, 0) < v:
                need[k] = v
        for b in reads:
            add(b.w, b.small)
        for b in writes:
            add(b.w, b.small)
            for d in b.r:
                add(d, b.small)
        out = []
        for k, v in need.items():
            if self.seen[e].get(k, 0) < v:
                self.seen[e][k] = v
                out.append((k, v))
        return out

    def op(self, e, fn, reads=(), writes=(), signal=True):
        waits = self._deps(e, reads, writes)
        val = self.cnt[e] + 1
        if signal:
            self.cnt[e] = val
        me = (e, val)
        for b in reads:
            b.r.append(me)
        for b in writes:
            b.w = me
            b.r = []
        self.q[e].append((waits, fn, signal, None))

    def dma(self, e, fn, semname, reads=(), writes=()):
        if semname not in self.dsem:
            self.dsem[semname] = [self.stack.enter_context(self.nc.semaphore("d_" + semname)), 0]
        waits = self._deps(e, reads, writes)
        self.dsem[semname][1] += 16
        me = (semname, self.dsem[semname][1])
        for b in reads:
            b.r.append(me)
        for b in writes:
            b.w = me
            b.r = []
        self.q[e].append((waits, fn, False, semname))

    def barrier(self):
        targets = [(k, self.cnt[k]) for k in ENGS if self.cnt[k] > 0]
        targets += [(k, v[1]) for k, v in self.dsem.items() if v[1] > 0]
        for e in ENGS:
            waits = []
            for k, v in targets:
                if k == e:
                    continue
                if self.seen[e].get(k, 0) < v:
                    self.seen[e][k] = v
                    waits.append((k, v))
            if waits:
                self.q[e].append((waits, None, False, None))

    def flush(self):
        for e in ENGS:
            items = self.q[e]
            if not items:
                continue
            self.q[e] = []
            semE = self.sem[e]

            def body(eng, items=items, semE=semE):
                for waits, fn, signal, dsem in items:
                    for k, v in waits:
                        eng.wait_ge(self._semh(k), v)
                    if fn is None:
                        continue
                    ins = fn(eng)
                    if dsem is not None:
                        ins.then_inc(self.dsem[dsem][0], 16)
                    elif signal:
                        ins.then_inc(semE, 1)
            getattr(self.block, BLK[e])(body)


def build_program(debug=False):
    nc = bass.Bass("TRN2", target_bir_lowering=False)

    def din(name, shape, dt=F32):
        return nc.dram_tensor(name, list(shape), dt, kind="ExternalInput").ap()

    x = din("x", [S, D])
    xT = din("xT", [NT, 128, 16, 128])
    mem = din("mem", [256, D])
    g1T = din("g1T", [128, 16])
    w_in = din("w_in", [D, 2880])
    gvb = din("gvb", [128, 1024]); bvb = din("bvb", [128, 1024]); gab = din("gab", [128, 1024])
    wsT = din("wsT", [128, 8, 128]); bspT = din("bspT", [128, 8])
    gqb = din("gqb", [128, 512]); gkvb = din("gkvb", [128, 256])
    w_uq = din("w_uq", [512, 8, 256])
    w_ukv = din("w_ukv", [256, 2048])
    gm = din("gm", [128, 8])
    w_out = din("w_out", [D, D])
    gxb = din("gxb", [128, D]); gmemb = din("gmemb", [128, D]); gmoeb = din("gmoeb", [128, D]); gfb = din("gfb", [128, D])
    w_mq = din("w_mq", [D, 512]); w_mk = din("w_mk", [D, 512]); w_mv = din("w_mv", [D, 512]); w_mo = din("w_mo", [512, D])
    wr = din("wr", [D, 72]); brb = din("brb", [128, 72])
    w_g = din("w_g", [NEXP, 128, 16 * 512]); w_u = din("w_u", [NEXP, 128, 16 * 512]); w_d = din("w_d", [NEXP, 128, 4 * D])
    cs_tok = din("cs_tok", [S, 64]); sn_tok = din("sn_tok", [S, 64])
    csT = din("csT", [64, S]); snT = din("snT", [64, S])
    consts = din("consts", [128, 5, 128])
    iota64 = din("iota64", [128, 64]); dummy = din("dummyrow", [128, 1])
    y = nc.dram_tensor("y", [S, D], F32, kind="ExternalOutput").ap()
    skind = "ExternalOutput" if debug else "Internal"
    X1 = nc.dram_tensor("X1", [S, D], F32, kind=skind).ap()
    X2 = nc.dram_tensor("X2", [S, D], F32, kind=skind).ap()
    Xbuf = nc.dram_tensor("Xbuf", [NSLOT, D], BF16, kind="Internal").ap()
    Ybuf = nc.dram_tensor("Ybuf", [NSLOT, D], BF16, kind="Internal").ap()
    if debug:
        dbgM = nc.dram_tensor("dbgM", [128, 16, S], BF16, kind="ExternalOutput").ap()
        dbgS = nc.dram_tensor("dbgS", [128, NT, 4], F32, kind="ExternalOutput").ap()

    with contextlib.ExitStack() as top:
        def sbt(st, name, shape, dt):
            return st.enter_context(nc.sbuf_tensor(name, list(shape), dt))

        banks = [top.enter_context(nc.psum_tensor("pb%d" % i, [128, 512], F32)) for i in range(8)]
        PB = bufs(8)
        cst = sbt(top, "cst", [128, 5, 128], F32)
        identb = sbt(top, "identb", [128, 128], BF16)
        maskb = sbt(top, "maskb", [128, 128], BF16)
        ltrib = sbt(top, "ltrib", [128, 128], BF16)
        onesb = sbt(top, "onesb", [128, 128], BF16)
        rstd1 = sbt(top, "rstd1", [128, NT], F32)
        slots = sbt(top, "slots", [128, NT, 2], I32)
        gates = sbt(top, "gates", [128, NT, 2], F32)
        neghalf = sbt(top, "neghalf", [128, 1], F32)
        Bnh = Buf()
        block = top.enter_context(nc.Block())
        P = Prog(nc, block, top)
        Bcst, Bident, Bmask, Bltri, Bones = bufs(5)
        Brstd1 = sbufs(NT)
        BaT = bufs(NT)
        BmT = bufs(8 * 4)
        Bslots = sbufs(NT)
        BX1 = bufs(NT); BX2 = bufs(NT); BXb = Buf(); BYb = Buf()

        identf = cst[:, 0, :]
        mid = contextlib.ExitStack()
        aT = sbt(mid, "aT", [128, 8, S], BF16)

        def bank_bf(i):
            return banks[i][:].bitcast(BF16)

        P.dma("sp", lambda e: e.dma_start(out=cst[:], in_=consts[:]), "cst", writes=[Bcst])
        P.op("dve", lambda e: e.tensor_copy(out=identb[:], in_=cst[:, 0, :]), reads=[Bcst], writes=[Bident])
        P.op("dve", lambda e: e.tensor_copy(out=maskb[:], in_=cst[:, 1, :]), reads=[Bcst], writes=[Bmask])
        P.op("dve", lambda e: e.tensor_copy(out=ltrib[:], in_=cst[:, 2, :]), reads=[Bcst], writes=[Bltri])
        P.op("dve", lambda e: e.tensor_copy(out=onesb[:], in_=cst[:, 3, :]), reads=[Bcst], writes=[Bones])
        P.op("dve", lambda e: e.memset(neghalf[:], -0.5), writes=[Bnh])

        def rstd_from_ss(st_tiles, ss_ap, Bss, out_ap, Bout, n, eps):
            tmp, Btmp = st_tiles
            P.op("pool", lambda e: e.tensor_scalar(out=tmp, in0=ss_ap, scalar1=1.0 / n, scalar2=eps, op0=ALU.mult, op1=ALU.add),
                 reads=[Bss], writes=[Btmp])
            P.op("pool", lambda e: e.tensor_tensor(out=out_ap, in0=tmp, in1=neghalf[:, 0:1], op=ALU.pow), reads=[Btmp, Bnh], writes=[Bout])

        def cast_load(dst, src, sem, Bdst):
            P.dma("pool", lambda e: e.dma_start(out=dst, in_=src), sem, writes=[Bdst])

        with contextlib.ExitStack() as st:
            w_uv = sbt(st, "w_uv", [128, 16, 2048], BF16)
            Bwuv = bufs(4)
            wv = w_in.rearrange("(kc p) n -> p kc n", p=128)
            for cb in range(4):
                for hh in range(2):
                    cast_load(w_uv[:, hh * 8:(hh + 1) * 8, cb * 512:(cb + 1) * 512], wv[:, hh * 8:(hh + 1) * 8, cb * 512:(cb + 1) * 512], "wuv%d" % cb, Bwuv[cb])
            g1 = sbt(st, "g1", [128, 16], F32); Bg1 = Buf()
            gv_t = sbt(st, "gv_t", [128, 1024], F32); bv_t = sbt(st, "bv_t", [128, 1024], F32); ga_t = sbt(st, "ga_t", [128, 1024], F32)
            wsf = sbt(st, "wsf", [128, 8, 128], F32); wsb = sbt(st, "wsb", [128, 8, 128], BF16); bsp = sbt(st, "bsp", [128, 8], F32)
            Bgv, Bbv, Bga, Bwsf, Bwsb, Bbsp = bufs(6)
            P.dma("sp", lambda e: e.dma_start(out=g1[:], in_=g1T[:]), "c0", writes=[Bg1])
            P.dma("sp", lambda e: e.dma_start(out=gv_t[:], in_=gvb[:]), "c1", writes=[Bgv])
            P.dma("sp", lambda e: e.dma_start(out=bv_t[:], in_=bvb[:]), "c2", writes=[Bbv])
            P.dma("sp", lambda e: e.dma_start(out=ga_t[:], in_=gab[:]), "c3", writes=[Bga])
            P.dma("sp", lambda e: e.dma_start(out=wsf[:], in_=wsT[:]), "c4", writes=[Bwsf])
            P.dma("sp", lambda e: e.dma_start(out=bsp[:], in_=bspT[:]), "c5", writes=[Bbsp])
            P.op("dve", lambda e: e.tensor_tensor(out=wsb[:], in0=wsf[:], in1=cst[:, 1, :].unsqueeze(1).to_broadcast([128, 8, 128]), op=ALU.mult),
                 reads=[Bwsf, Bcst], writes=[Bwsb])
            xt = sbt(st, "xt0", [128, D], F32); Bxt = Buf()
            xTt = [sbt(st, "xTt%d" % k, [128, 16, 128], F32) for k in range(2)]
            xgT = [sbt(st, "xgT%d" % k, [128, 16, 128], BF16) for k in range(2)]
            BxTt, BxgT = bufs(2), bufs(2)
            u = [sbt(st, "u%d" % k, [128, 1024], BF16) for k in range(2)]
            v = [sbt(st, "v%d" % k, [128, 1024], F32) for k in range(2)]
            vn = [sbt(st, "vn%d" % k, [128, 1024], BF16) for k in range(2)]
            junk = sbt(st, "junk", [128, D], BF16); Bjunk = Buf()
            stt = [sbt(st, "stt%d" % k, [128, 16], F32) for k in range(2)]
            Bu, Bv, Bvn = bufs(2), bufs(2), bufs(2)
            Bst2 = [sbufs(16), sbufs(16)]

            def a1_pre(i):
                k = i % 2
                st_, Bst = stt[k], Bst2[k]
                P.dma("sp", lambda e, i=i: e.dma_start(out=xt[:], in_=x[i * 128:(i + 1) * 128, :]), "xt0", writes=[Bxt])
                P.dma("sp", lambda e, i=i, k=k: e.dma_start(out=xTt[k][:], in_=xT[i]), "xTt%d" % k, writes=[BxTt[k]])
                P.op("act", lambda e: e.activation(out=junk[:], in_=xt[:], func=AF.Square, accum_out=st_[:, 0:1]), reads=[Bxt], writes=[Bjunk, Bst[0]])
                rstd_from_ss((st_[:, 1:2], Bst[1]), st_[:, 0:1], Bst[0], rstd1[:, i:i + 1], Brstd1[i], D, EPS)
                P.op("dve", lambda e, k=k: e.tensor_tensor(out=xgT[k][:], in0=xTt[k][:], in1=g1[:, :].unsqueeze(2).to_broadcast([128, 16, 128]), op=ALU.mult),
                     reads=[BxTt[k], Bg1], writes=[BxgT[k]])

            def a1_mm_pe(i, cbs):
                k = i % 2
                for cb in cbs:
                    bk = cb
                    for kc in range(16):
                        P.op("pe", lambda e, k=k, kc=kc, cb=cb, bk=bk: e.matmul(banks[bk][:], xgT[k][:, kc, :], w_uv[:, kc, cb * 512:(cb + 1) * 512], start=(kc == 0), stop=(kc == 15)),
                             reads=[BxgT[k], Bwuv[cb]], writes=[PB[bk]], signal=(kc == 15))

            def a1_gelu(i):
                k = i % 2
                st_, Bst = stt[k], Bst2[k]
                for cb in range(4):
                    bk = cb
                    if cb < 2:
                        P.op("act", lambda e, cb=cb, bk=bk, i=i, k=k: e.activation(out=u[k][:, cb * 512:(cb + 1) * 512], in_=banks[bk][:], func=AF.Gelu_apprx_tanh, scale=rstd1[:, i:i + 1]),
                             reads=[PB[bk], Brstd1[i]], writes=[Bu[k]])
                    else:
                        c2 = cb - 2
                        P.op("act", lambda e, c2=c2, bk=bk, i=i, k=k: e.activation(out=v[k][:, c2 * 512:(c2 + 1) * 512], in_=banks[bk][:], func=AF.Gelu_apprx_tanh, scale=rstd1[:, i:i + 1],
                                                                              accum_out=st_[:, 2 + c2:3 + c2]),
                             reads=[PB[bk], Brstd1[i]], writes=[Bv[k], Bst[2 + c2]])

            def a1_tail1(i):
                k = i % 2
                st_, Bst = stt[k], Bst2[k]
                vk, uk, vnk = v[k], u[k], vn[k]
                P.op("act", lambda e: e.activation(out=junk[:, 0:1024], in_=vk[:], func=AF.Square, accum_out=st_[:, 4:5]), reads=[Bv[k]], writes=[Bjunk, Bst[4]])
                P.op("dve", lambda e: e.tensor_scalar(out=st_[:, 5:6], in0=st_[:, 2:3], scalar1=st_[:, 3:4], scalar2=1.0 / 1024, op0=ALU.add, op1=ALU.mult),
                     reads=[Bst[2], Bst[3]], writes=[Bst[5]])
                P.op("dve", lambda e: e.tensor_tensor(out=st_[:, 6:7], in0=st_[:, 5:6], in1=st_[:, 5:6], op=ALU.mult), reads=[Bst[5]], writes=[Bst[6]])
                P.op("dve", lambda e: e.scalar_tensor_tensor(out=st_[:, 7:8], in0=st_[:, 4:5], scalar=1.0 / 1024, in1=st_[:, 6:7], op0=ALU.mult, op1=ALU.subtract),
                     reads=[Bst[4], Bst[6]], writes=[Bst[7]])
                P.op("dve", lambda e: e.tensor_scalar(out=st_[:, 8:9], in0=st_[:, 7:8], scalar1=1e-5, scalar2=None, op0=ALU.add), reads=[Bst[7]], writes=[Bst[8]])
                P.op("pool", lambda e: e.tensor_tensor(out=st_[:, 9:10], in0=st_[:, 8:9], in1=neghalf[:, 0:1], op=ALU.pow), reads=[Bst[8], Bnh], writes=[Bst[9]])
                P.op("dve", lambda e: e.tensor_scalar(out=vk[:], in0=vk[:], scalar1=st_[:, 5:6], scalar2=st_[:, 9:10], op0=ALU.subtract, op1=ALU.mult),
                     reads=[Bv[k], Bst[5], Bst[9]], writes=[Bv[k]])
                P.op("dve", lambda e: e.tensor_tensor(out=vk[:], in0=vk[:], in1=gv_t[:], op=ALU.mult), reads=[Bv[k], Bgv], writes=[Bv[k]])
                P.op("dve", lambda e: e.tensor_tensor(out=vnk[:], in0=vk[:], in1=bv_t[:], op=ALU.add), reads=[Bv[k], Bbv], writes=[Bvn[k]])
                for g in range(8):
                    bk = 4 + g // 4
                    P.op("pe", lambda e, g=g, bk=bk: e.matmul(banks[bk][:, (g % 4) * 128:(g % 4 + 1) * 128], wsb[:, g, :], vnk[:, g * 128:(g + 1) * 128], start=True, stop=True),
                         reads=[Bwsb, Bvn[k]], writes=[PB[bk]], signal=(g % 4 == 3))

            def a1_tail2(i):
                k = i % 2
                st_, Bst = stt[k], Bst2[k]
                vk, uk, vnk = v[k], u[k], vn[k]
                for g in range(8):
                    bk = 4 + g // 4
                    P.op("dve", lambda e, g=g, bk=bk: e.scalar_tensor_tensor(out=vk[:, g * 128:(g + 1) * 128], in0=banks[bk][:, (g % 4) * 128:(g % 4 + 1) * 128],
                                                                            scalar=bsp[:, g:g + 1], in1=uk[:, g * 128:(g + 1) * 128], op0=ALU.add, op1=ALU.mult),
                         reads=[PB[bk], Bbsp, Bu[k]], writes=[Bv[k]])
                P.op("act", lambda e: e.activation(out=junk[:, 0:1024], in_=vk[:], func=AF.Square, accum_out=st_[:, 10:11]), reads=[Bv[k]], writes=[Bjunk, Bst[10]])
                rstd_from_ss((st_[:, 11:12], Bst[11]), st_[:, 10:11], Bst[10], st_[:, 12:13], Bst[12], 1024, EPS)
                P.op("dve", lambda e: e.scalar_tensor_tensor(out=vnk[:], in0=vk[:], scalar=st_[:, 12:13], in1=ga_t[:], op0=ALU.mult, op1=ALU.mult),
                     reads=[Bv[k], Bst[12], Bga], writes=[Bvn[k]])
                for c in range(8):
                    P.op("pe", lambda e, c=c: e.transpose(out=bank_bf(6)[:, c * 128:(c + 1) * 128], in_=vnk[:, c * 128:(c + 1) * 128], identity=identb[:]),
                         reads=[Bvn[k], Bident], writes=[PB[6]], signal=(c == 7))
                P.op("act", lambda e, i=i: e.activation(out=aT[:, :, i * 128:(i + 1) * 128], in_=bank_bf(6).rearrange("p (c t) -> p c t", c=8), func=AF.Copy),
                     reads=[PB[6]], writes=[BaT[i]])

            a1_pre(0)
            a1_mm_pe(0, range(4))
            a1_gelu(0)
            a1_pre(1)
            for i in range(NT):
                if i + 1 < NT:
                    a1_mm_pe(i + 1, (0, 1))
                a1_tail1(i)
                if i + 1 < NT:
                    a1_mm_pe(i + 1, (2, 3))
                if i + 2 < NT:
                    a1_pre(i + 2)
                a1_tail2(i)
                if i + 1 < NT:
                    a1_gelu(i + 1)
            P.barrier()
            P.flush()

        mTn = sbt(mid, "mTn", [128, 8, S], BF16)
        with contextlib.ExitStack() as st:
            cqT = sbt(st, "cqT", [128, 4, S], BF16); ckvT = sbt(st, "ckvT", [128, 2, S], BF16); krT = sbt(st, "krT", [64, S], BF16)
            BcqT, BckvT, BkrT = bufs(NT), bufs(NT), bufs(NT)
            with contextlib.ExitStack() as st2:
                w_lat = sbt(st2, "w_lat", [128, 16, 832], BF16); Bwlat = Buf()
                wv = w_in.rearrange("(kc p) n -> p kc n", p=128)
                for hh in range(2):
                    cast_load(w_lat[:, hh * 8:(hh + 1) * 8, :], wv[:, hh * 8:(hh + 1) * 8, 2048:2880], "wlat", Bwlat)
                g1 = sbt(st2, "g1b", [128, 16], F32); Bg1 = Buf()
                gq_t = sbt(st2, "gq_t", [128, 512], F32); gkv_t = sbt(st2, "gkv_t", [128, 256], F32); Bgq, Bgkv = bufs(2)
                P.dma("sp", lambda e: e.dma_start(out=g1[:], in_=g1T[:]), "c0", writes=[Bg1])
                P.dma("sp", lambda e: e.dma_start(out=gq_t[:], in_=gqb[:]), "c1", writes=[Bgq])
                P.dma("sp", lambda e: e.dma_start(out=gkv_t[:], in_=gkvb[:]), "c2", writes=[Bgkv])
                xTt = [sbt(st2, "xTu%d" % k, [128, 16, 128], F32) for k in range(2)]
                xgT = [sbt(st2, "xgU%d" % k, [128, 16, 128], BF16) for k in range(2)]
                cst_k = [sbt(st2, "cstk%d" % k, [128, 2, 64], F32) for k in range(2)]
                BxTt, BxgT, Bcsk = bufs(2), bufs(2), bufs(2)
                lat = [sbt(st2, "lat%d" % k, [128, 832], F32) for k in range(2)]
                latn = [sbt(st2, "latn%d" % k, [128, 832], BF16) for k in range(2)]
                junk = sbt(st2, "junkb", [128, 512], BF16)
                krtmp = [sbt(st2, "krtmp%d" % k, [128, 2, 64], F32) for k in range(2)]
                stt = [sbt(st2, "sttb%d" % k, [128, 8], F32) for k in range(2)]
                Blat, Blatn, Bkrtmp = bufs(2), bufs(2), bufs(2)
                Bjunk = Buf()
                Bst2 = [sbufs(8), sbufs(8)]

                def a2_pre(i):
                    k = i % 2
                    P.dma("sp", lambda e: e.dma_start(out=xTt[k][:], in_=xT[i]), "xTt%d" % k, writes=[BxTt[k]])
                    P.op("dve", lambda e: e.tensor_tensor(out=xgT[k][:], in0=xTt[k][:], in1=g1[:, :].unsqueeze(2).to_broadcast([128, 16, 128]), op=ALU.mult),
                         reads=[BxTt[k], Bg1], writes=[BxgT[k]])

                def a2_cs(i):
                    k = i % 2
                    P.dma("sp", lambda e: e.dma_start(out=cst_k[k][:, 0, :], in_=cs_tok[i * 128:(i + 1) * 128, :]), "csk%d" % k, writes=[Bcsk[k]])
                    P.dma("sp", lambda e: e.dma_start(out=cst_k[k][:, 1, :], in_=sn_tok[i * 128:(i + 1) * 128, :]), "csk%d" % k, writes=[Bcsk[k]])

                def a2_mm(i):
                    k = i % 2
                    b0, b1 = (0, 1) if k == 0 else (4, 5)
                    for kc in range(16):
                        P.op("pe", lambda e, kc=kc: e.matmul(banks[b0][:], xgT[k][:, kc, :], w_lat[:, kc, 0:512], start=(kc == 0), stop=(kc == 15)),
                             reads=[BxgT[k], Bwlat], writes=[PB[b0]], signal=(kc == 15))
                    for kc in range(16):
                        P.op("pe", lambda e, kc=kc: e.matmul(banks[b1][:, 0:320], xgT[k][:, kc, :], w_lat[:, kc, 512:832], start=(kc == 0), stop=(kc == 15)),
                             reads=[BxgT[k], Bwlat], writes=[PB[b1]], signal=(kc == 15))

                def a2_evac(i):
                    k = i % 2
                    b0, b1 = (0, 1) if k == 0 else (4, 5)
                    P.op("act", lambda e: e.activation(out=lat[k][:, 0:512], in_=banks[b0][:], func=AF.Copy, scale=rstd1[:, i:i + 1]), reads=[PB[b0], Brstd1[i]], writes=[Blat[k]])
                    P.op("act", lambda e: e.activation(out=lat[k][:, 512:832], in_=banks[b1][:, 0:320], func=AF.Copy, scale=rstd1[:, i:i + 1]), reads=[PB[b1], Brstd1[i]], writes=[Blat[k]])

                def a2_tail(i):
                    k = i % 2
                    st_, Bst = stt[k], Bst2[k]
                    la, ln, kt = lat[k], latn[k], krtmp[k]
                    P.op("act", lambda e: e.activation(out=junk[:, 0:512], in_=la[:, 0:512], func=AF.Square, accum_out=st_[:, 0:1]), reads=[Blat[k]], writes=[Bjunk, Bst[0]])
                    rstd_from_ss((st_[:, 1:2], Bst[1]), st_[:, 0:1], Bst[0], st_[:, 2:3], Bst[2], 512, EPS)
                    P.op("dve", lambda e: e.scalar_tensor_tensor(out=ln[:, 0:512], in0=la[:, 0:512], scalar=st_[:, 2:3], in1=gq_t[:], op0=ALU.mult, op1=ALU.mult),
                         reads=[Blat[k], Bst[2], Bgq], writes=[Blatn[k]])
                    P.op("act", lambda e: e.activation(out=junk[:, 0:256], in_=la[:, 512:768], func=AF.Square, accum_out=st_[:, 3:4]), reads=[Blat[k]], writes=[Bjunk, Bst[3]])
                    rstd_from_ss((st_[:, 4:5], Bst[4]), st_[:, 3:4], Bst[3], st_[:, 5:6], Bst[5], 256, EPS)
                    P.op("dve", lambda e: e.scalar_tensor_tensor(out=ln[:, 512:768], in0=la[:, 512:768], scalar=st_[:, 5:6], in1=gkv_t[:], op0=ALU.mult, op1=ALU.mult),
                         reads=[Blat[k], Bst[5], Bgkv], writes=[Blatn[k]])
                    P.op("dve", lambda e: e.tensor_tensor(out=kt[:, 0, :], in0=la[:, 768:832], in1=cst_k[k][:, 0, :], op=ALU.mult), reads=[Blat[k], Bcsk[k]], writes=[Bkrtmp[k]])
                    P.op("dve", lambda e: e.tensor_tensor(out=kt[:, 1, 0:32], in0=la[:, 800:832], in1=cst_k[k][:, 1, 0:32], op=ALU.mult), reads=[Blat[k], Bcsk[k]], writes=[Bkrtmp[k]])
                    P.op("dve", lambda e: e.tensor_tensor(out=kt[:, 1, 32:64], in0=la[:, 768:800], in1=cst_k[k][:, 1, 32:64], op=ALU.mult), reads=[Blat[k], Bcsk[k]], writes=[Bkrtmp[k]])
                    P.op("dve", lambda e: e.tensor_tensor(out=ln[:, 768:832], in0=kt[:, 0, :], in1=kt[:, 1, :], op=ALU.add), reads=[Bkrtmp[k]], writes=[Blatn[k]])
                    for c in range(4):
                        P.op("pe", lambda e, c=c: e.transpose(out=bank_bf(2)[:, c * 128:(c + 1) * 128], in_=ln[:, c * 128:(c + 1) * 128], identity=identb[:]),
                             reads=[Blatn[k], Bident], writes=[PB[2]], signal=(c == 3))
                    P.op("act", lambda e: e.activation(out=cqT[:, :, i * 128:(i + 1) * 128], in_=bank_bf(2)[:, 0:512].rearrange("p (c t) -> p c t", c=4), func=AF.Copy),
                         reads=[PB[2]], writes=[BcqT[i]])
                    for c in range(2):
                        P.op("pe", lambda e, c=c: e.transpose(out=bank_bf(3)[:, c * 128:(c + 1) * 128], in_=ln[:, 512 + c * 128:512 + (c + 1) * 128], identity=identb[:]),
                             reads=[Blatn[k], Bident], writes=[PB[3]], signal=False)
                    P.op("pe", lambda e: e.transpose(out=bank_bf(3)[0:64, 256:384], in_=ln[:, 768:832], identity=identb[:]),
                         reads=[Blatn[k], Bident], writes=[PB[3]], signal=True)
                    P.op("dve", lambda e: e.tensor_copy(out=ckvT[:, :, i * 128:(i + 1) * 128], in_=bank_bf(3)[:, 0:256].rearrange("p (c t) -> p c t", c=2)),
                         reads=[PB[3]], writes=[BckvT[i]])
                    P.op("dve", lambda e: e.tensor_copy(out=krT[:, i * 128:(i + 1) * 128], in_=bank_bf(3)[0:64, 256:384]),
                         reads=[PB[3]], writes=[BkrT[i]])

                a2_cs(0)
                a2_cs(1)
                a2_pre(0)
                a2_mm(0)
                a2_evac(0)
                a2_pre(1)
                for i in range(NT):
                    if i + 1 < NT:
                        a2_mm(i + 1)
                    if i + 2 < NT:
                        a2_pre(i + 2)
                    a2_tail(i)
                    if i + 2 < NT:
                        a2_cs(i + 2)
                    if i + 1 < NT:
                        a2_evac(i + 1)
                P.barrier()
                P.flush()
            wuq = sbt(st, "wuq", [128, 4, 8, 256], BF16); wukv = sbt(st, "wukv", [128, 2, 2048], BF16); Bwuq, Bwukv = bufs(2)
            for hh in range(2):
                cast_load(wuq[:, :, hh * 4:(hh + 1) * 4, :], w_uq.rearrange("(kc p) h n -> p kc h n", p=128)[:, :, hh * 4:(hh + 1) * 4, :], "wuq", Bwuq)
                cast_load(wukv[:, :, hh * 1024:(hh + 1) * 1024], w_ukv.rearrange("(kc p) n -> p kc n", p=128)[:, :, hh * 1024:(hh + 1) * 1024], "wukv", Bwukv)
            cs_f = sbt(st, "cs_f", [64, S], F32); sn_f = sbt(st, "sn_f", [64, S], F32); gm_t = sbt(st, "gm_t", [128, 8], F32)
            Bcsf, Bsnf, Bgm = bufs(3)
            P.dma("sp", lambda e: e.dma_start(out=cs_f[:], in_=csT[:]), "c0", writes=[Bcsf])
            P.dma("sp", lambda e: e.dma_start(out=sn_f[:], in_=snT[:]), "c1", writes=[Bsnf])
            P.dma("sp", lambda e: e.dma_start(out=gm_t[:], in_=gm[:]), "c2", writes=[Bgm])
            QnT = [sbt(st, "QnT%d" % k, [128, S], BF16) for k in range(2)]
            QrT = [sbt(st, "QrT%d" % k, [64, S], BF16) for k in range(2)]
            KnT = [sbt(st, "KnT%d" % k, [128, S], BF16) for k in range(2)]
            Vh = [sbt(st, "Vh%d" % k, [128, NT, 128], BF16) for k in range(2)]
            BQn, BQr, BKn, BVh = bufs(2), bufs(2), bufs(2), bufs(2)
            rt1 = sbt(st, "rt1", [64, 512], F32); rt2 = sbt(st, "rt2", [64, 512], F32); Brt1, Brt2 = bufs(2)
            Pt = [sbt(st, "Pt%d" % k, [128, 512], BF16) for k in range(3)]; BPt = bufs(3)
            rec = sbt(st, "rec", [128, 512], F32); Brec = Buf()
            scale = 1.0 / math.sqrt(192.0)
            allT = lambda B_: list(B_)
            pt_i = 0
            s_i = 0
            def b_prep(h):
                k = h % 2
                for G in range(4):
                    gs = slice(G * 512, (G + 1) * 512)
                    tl = [4 * G + t for t in range(4)]
                    for kc in range(4):
                        P.op("pe", lambda e, kc=kc, h=h, gs=gs: e.matmul(banks[0][:], wuq[:, kc, h, 0:128], cqT[:, kc, gs], start=(kc == 0), stop=(kc == 3)),
                             reads=[Bwuq] + [BcqT[t] for t in tl], writes=[PB[0]], signal=(kc == 3))
                    P.op("act", lambda e, k=k, gs=gs: e.activation(out=QnT[k][:, gs], in_=banks[0][:], func=AF.Copy), reads=[PB[0]], writes=[BQn[k]])
                    for kc in range(4):
                        P.op("pe", lambda e, kc=kc, h=h, gs=gs: e.matmul(banks[1][0:64, :], wuq[:, kc, h, 128:192], cqT[:, kc, gs], start=(kc == 0), stop=(kc == 3)),
                             reads=[Bwuq] + [BcqT[t] for t in tl], writes=[PB[1]], signal=(kc == 3))
                    for kc in range(4):
                        P.op("pe", lambda e, kc=kc, h=h, gs=gs: e.matmul(banks[2][0:64, :], wuq[:, kc, h, 192:256], cqT[:, kc, gs], start=(kc == 0), stop=(kc == 3)),
                             reads=[Bwuq] + [BcqT[t] for t in tl], writes=[PB[2]], signal=(kc == 3))
                    P.op("dve", lambda e, gs=gs: e.tensor_tensor(out=rt1[:], in0=banks[1][0:64, :], in1=cs_f[:, gs], op=ALU.mult), reads=[PB[1], Bcsf], writes=[Brt1])
                    P.op("dve", lambda e, gs=gs: e.tensor_tensor(out=rt2[:], in0=banks[2][0:64, :], in1=sn_f[:, gs], op=ALU.mult), reads=[PB[2], Bsnf], writes=[Brt2])
                    P.op("dve", lambda e, k=k, gs=gs: e.tensor_tensor(out=QrT[k][:, gs], in0=rt1[:], in1=rt2[:], op=ALU.add), reads=[Brt1, Brt2], writes=[BQr[k]])
                    for kc in range(2):
                        P.op("pe", lambda e, kc=kc, h=h, gs=gs: e.matmul(banks[3][:], wukv[:, kc, h * 256:h * 256 + 128], ckvT[:, kc, gs], start=(kc == 0), stop=(kc == 1)),
                             reads=[Bwukv] + [BckvT[t] for t in tl], writes=[PB[3]], signal=(kc == 1))
                    P.op("act", lambda e, k=k, gs=gs: e.activation(out=KnT[k][:, gs], in_=banks[3][:], func=AF.Copy), reads=[PB[3]], writes=[BKn[k]])
                    for t in range(4):
                        j = 4 * G + t
                        for kc in range(2):
                            P.op("pe", lambda e, kc=kc, h=h, j=j, t=t: e.matmul(banks[0][:, t * 128:(t + 1) * 128], ckvT[:, kc, j * 128:(j + 1) * 128], wukv[:, kc, h * 256 + 128:h * 256 + 256], start=(kc == 0), stop=(kc == 1)),
                                 reads=[Bwukv, BckvT[j]], writes=[PB[0]], signal=(kc == 1 and t == 3))
                    P.op("dve", lambda e, k=k, G=G: e.tensor_copy(out=Vh[k][:, 4 * G:4 * G + 4, :], in_=banks[0][:].rearrange("p (t d) -> p t d", t=4)), reads=[PB[0]], writes=[BVh[k]])

            cnt = {"s": 0, "p": 0}

            def b_S(h, G, j):
                k = h % 2
                lo = max(0, j * 128 - G * 512)
                q0 = G * 512 + lo
                q1 = (G + 1) * 512
                sb_ = 1 + (cnt["s"] % 3); cnt["s"] += 1
                pi = cnt["p"] % 3; cnt["p"] += 1
                P.op("pe", lambda e: e.matmul(banks[sb_][:, lo:512], KnT[k][:, j * 128:(j + 1) * 128], QnT[k][:, q0:q1], start=True, stop=False),
                     reads=[BKn[k], BQn[k]], writes=[PB[sb_]], signal=False)
                P.op("pe", lambda e: e.matmul(banks[sb_][:, lo:512], krT[:, j * 128:(j + 1) * 128], QrT[k][:, q0:q1], start=False, stop=True),
                     reads=[BkrT[j], BQr[k]], writes=[PB[sb_]], signal=True)
                P.op("act", lambda e: e.activation(out=Pt[pi][:, lo:512], in_=banks[sb_][:, lo:512], func=AF.Exp, scale=scale),
                     reads=[PB[sb_]], writes=[BPt[pi]])
                if j >= 4 * G:
                    P.op("dve", lambda e: e.tensor_tensor(out=Pt[pi][:, lo:lo + 128], in0=Pt[pi][:, lo:lo + 128], in1=maskb[:], op=ALU.mult),
                         reads=[BPt[pi], Bmask], writes=[BPt[pi]])
                return (lo, pi)

            def b_PV(h, G, j, lo, pi):
                k = h % 2
                ob, rb = 4 + (G % 2), 6 + (G % 2)
                nj = 4 * G + 4
                P.op("pe", lambda e: e.matmul(banks[ob][:, lo:512], Vh[k][:, j, :], Pt[pi][:, lo:512], start=(j == 0), stop=(j == nj - 1)),
                     reads=[BVh[k], BPt[pi]], writes=[PB[ob]], signal=False)
                P.op("pe", lambda e: e.matmul(banks[rb][:, lo:512], onesb[:], Pt[pi][:, lo:512], start=(j == 0), stop=(j == nj - 1)),
                     reads=[Bones, BPt[pi]], writes=[PB[rb]], signal=True)
                if j == nj - 1:
                    P.op("dve", lambda e: e.reciprocal(out=rec[:], in_=banks[rb][:]), reads=[PB[rb]], writes=[Brec])
                    P.op("dve", lambda e: e.tensor_tensor(out=mTn[:, h, G * 512:(G + 1) * 512], in0=banks[ob][:], in1=rec[:], op=ALU.mult),
                         reads=[PB[ob], Brec], writes=[BmT[h * 4 + G]])

            b_prep(0)
            for h in range(8):
                if h + 1 < 8:
                    b_prep(h + 1)
                its = [(G, j) for G in range(4) for j in range(4 * G + 4)]
                pend = b_S(h, *its[0])
                for n, (G, j) in enumerate(its):
                    nxt = b_S(h, *its[n + 1]) if n + 1 < len(its) else None
                    b_PV(h, G, j, *pend)
                    pend = nxt
            sq = [sbt(st, "sq%d" % k, [128, 512], BF16) for k in range(2)]; Bsq = bufs(2)
            rbc = sbt(st, "rbc", [128, 512], F32); Brbc = Buf()
            for G in range(4):
                gs = slice(G * 512, (G + 1) * 512)
                for h in range(8):
                    k = h % 2
                    P.op("act", lambda e, k=k, h=h, gs=gs: e.activation(out=sq[k][:], in_=mTn[:, h, gs], func=AF.Square), reads=[BmT[h * 4 + G]], writes=[Bsq[k]])
                    P.op("pe", lambda e, k=k, h=h: e.matmul(banks[0][:], onesb[:], sq[k][:], start=(h == 0), stop=(h == 7)), reads=[Bones, Bsq[k]], writes=[PB[0]], signal=True)
                P.op("dve", lambda e: e.tensor_scalar(out=rbc[:], in0=banks[0][:], scalar1=1.0 / 1024, scalar2=EPS, op0=ALU.mult, op1=ALU.add), reads=[PB[0]], writes=[Brbc])
                P.op("act", lambda e: e.activation(out=rbc[:], in_=rbc[:], func=AF.Sqrt), reads=[Brbc], writes=[Brbc])
                P.op("dve", lambda e: e.reciprocal(out=rbc[:], in_=rbc[:]), reads=[Brbc], writes=[Brbc])
                for h in range(8):
                    P.op("dve", lambda e, h=h, gs=gs: e.scalar_tensor_tensor(out=mTn[:, h, gs], in0=mTn[:, h, gs], scalar=gm_t[:, h:h + 1], in1=rbc[:], op0=ALU.mult, op1=ALU.mult),
                         reads=[Bgm, Brbc], writes=[BmT[h * 4 + G]])
            P.barrier()
            P.flush()

        if debug:
            P.dma("sp", lambda e: e.dma_start(out=dbgM[:, 0:8, :], in_=aT[:]), "dbg", reads=BaT)
            P.dma("sp", lambda e: e.dma_start(out=dbgM[:, 8:16, :], in_=mTn[:]), "dbg", reads=BmT)
        with contextlib.ExitStack() as st:
            wo = sbt(st, "wo", [128, 16, D], BF16); Bwo = bufs(4)
            wov = w_out.rearrange("(kc p) n -> p kc n", p=128)
            for cb in range(4):
                for hh in range(2):
                    cast_load(wo[:, hh * 8:(hh + 1) * 8, cb * 512:(cb + 1) * 512], wov[:, hh * 8:(hh + 1) * 8, cb * 512:(cb + 1) * 512], "wo%d" % cb, Bwo[cb])
            xt = [sbt(st, "xc%d" % k, [128, D], F32) for k in range(2)]; Bxt = bufs(2)
            for i in range(NT):
                k = i % 2
                ts_ = slice(i * 128, (i + 1) * 128)
                P.dma("sp", lambda e, k=k, ts_=ts_: e.dma_start(out=xt[k][:], in_=x[ts_, :]), "xt%d" % k, writes=[Bxt[k]])
                for cb in range(4):
                    bk = (i % 2) * 4 + cb
                    for kc in range(16):
                        rd = [BaT[i]] if kc < 8 else [BmT[(kc - 8) * 4 + i // 4]]
                        P.op("pe", lambda e, kc=kc, cb=cb, bk=bk, ts_=ts_: e.matmul(banks[bk][:], (aT[:, kc, ts_] if kc < 8 else mTn[:, kc - 8, ts_]), wo[:, kc, cb * 512:(cb + 1) * 512], start=(kc == 0), stop=(kc == 15)),
                             reads=rd + [Bwo[cb]], writes=[PB[bk]], signal=(kc == 15))
                    P.op("dve", lambda e, k=k, cb=cb, bk=bk: e.tensor_tensor(out=xt[k][:, cb * 512:(cb + 1) * 512], in0=banks[bk][:], in1=xt[k][:, cb * 512:(cb + 1) * 512], op=ALU.add),
                         reads=[PB[bk], Bxt[k]], writes=[Bxt[k]])
                P.dma("sp", lambda e, k=k, ts_=ts_: e.dma_start(out=X1[ts_, :], in_=xt[k][:]), "x1w%d" % k, reads=[Bxt[k]], writes=[BX1[i]])
            P.barrier()
            P.flush()
        mid.close()
        with contextlib.ExitStack() as st:
            wmq = sbt(st, "wmq", [128, 16, 512], BF16); wmo = sbt(st, "wmo", [128, 4, D], BF16); Bwmq, Bwmo = bufs(2)
            memKT = sbt(st, "memKT", [128, 4, 256], BF16); memV = sbt(st, "memV", [128, 2, 512], BF16); BmemK, BmemV = bufs(2)
            gx_t = sbt(st, "gx_t", [128, D], F32); gmoe_t = sbt(st, "gmoe_t", [128, D], F32); Bgx, Bgmoe = bufs(2)
            wr_t = sbt(st, "wr_t", [128, 16, 72], F32); br_t = sbt(st, "br_t", [128, 72], F32); io_t = sbt(st, "io_t", [128, 64], F32); dm_t = sbt(st, "dm_t", [128, 1], F32)
            Bwr, Bbr, Bio, Bdm = bufs(4)
            for hh in range(2):
                cast_load(wmq[:, hh * 8:(hh + 1) * 8, :], w_mq.rearrange("(kc p) n -> p kc n", p=128)[:, hh * 8:(hh + 1) * 8, :], "wmq", Bwmq)
                cast_load(wmo[:, :, hh * 1024:(hh + 1) * 1024], w_mo.rearrange("(kc p) n -> p kc n", p=128)[:, :, hh * 1024:(hh + 1) * 1024], "wmo", Bwmo)
            P.dma("sp", lambda e: e.dma_start(out=gx_t[:], in_=gxb[:]), "c0", writes=[Bgx])
            P.dma("sp", lambda e: e.dma_start(out=gmoe_t[:], in_=gmoeb[:]), "c1", writes=[Bgmoe])
            P.dma("sp", lambda e: e.dma_start(out=wr_t[:], in_=wr.rearrange("(kc p) n -> p kc n", p=128)), "c2", writes=[Bwr])
            P.dma("sp", lambda e: e.dma_start(out=br_t[:], in_=brb[:]), "c3", writes=[Bbr])
            P.dma("sp", lambda e: e.dma_start(out=io_t[:], in_=iota64[:]), "c4", writes=[Bio])
            P.dma("sp", lambda e: e.dma_start(out=dm_t[:], in_=dummy[:]), "c5", writes=[Bdm])
            xt = [sbt(st, "xd%d" % k, [128, D], F32) for k in range(3)]; Bxt = bufs(3)
            hb = sbt(st, "hb", [128, D], BF16); hT = sbt(st, "hT", [128, 16, 128], BF16); Bhb, BhT = bufs(2)
            junk = sbt(st, "junkc", [128, D], BF16); Bjunk = Buf()
            stt = sbt(st, "sttc", [128, 8], F32); Bst = sbufs(8)
            with contextlib.ExitStack() as st2:
                wmk = sbt(st2, "wmk", [128, 16, 512], BF16); wmv = sbt(st2, "wmv", [128, 16, 512], BF16); Bwmk, Bwmv = bufs(2)
                gmem_t = sbt(st2, "gmem_t", [128, D], F32); Bgmem = Buf()
                memT = sbt(st2, "memT", [128, 16, 256], BF16); BmemT = bufs(2)
                for hh in range(2):
                    cast_load(wmk[:, hh * 8:(hh + 1) * 8, :], w_mk.rearrange("(kc p) n -> p kc n", p=128)[:, hh * 8:(hh + 1) * 8, :], "wmk", Bwmk)
                    cast_load(wmv[:, hh * 8:(hh + 1) * 8, :], w_mv.rearrange("(kc p) n -> p kc n", p=128)[:, hh * 8:(hh + 1) * 8, :], "wmv", Bwmv)
                P.dma("sp", lambda e: e.dma_start(out=gmem_t[:], in_=gmemb[:]), "c6", writes=[Bgmem])
                for mt in range(2):
                    P.dma("sp", lambda e, mt=mt: e.dma_start(out=xt[mt][:], in_=mem[mt * 128:(mt + 1) * 128, :]), "xt%d" % mt, writes=[Bxt[mt]])
                    P.op("act", lambda e, mt=mt: e.activation(out=junk[:], in_=xt[mt][:], func=AF.Square, accum_out=stt[:, 0:1]), reads=[Bxt[mt]], writes=[Bjunk, Bst[0]])
                    rstd_from_ss((stt[:, 1:2], Bst[1]), stt[:, 0:1], Bst[0], stt[:, 2:3], Bst[2], D, EPS)
                    P.op("dve", lambda e, mt=mt: e.scalar_tensor_tensor(out=hb[:], in0=xt[mt][:], scalar=stt[:, 2:3], in1=gmem_t[:], op0=ALU.mult, op1=ALU.mult),
                         reads=[Bxt[mt], Bst[2], Bgmem], writes=[Bhb])
                    for half in range(2):
                        for c in range(8):
                            kc = half * 8 + c
                            P.op("pe", lambda e, c=c, kc=kc, half=half: e.transpose(out=bank_bf(half)[:, c * 128:(c + 1) * 128], in_=hb[:, kc * 128:(kc + 1) * 128], identity=identb[:]),
                                 reads=[Bhb, Bident], writes=[PB[half]], signal=(c == 7))
                        P.op("act", lambda e, half=half, mt=mt: e.activation(out=memT[:, half * 8:(half + 1) * 8, mt * 128:(mt + 1) * 128], in_=bank_bf(half).rearrange("p (c t) -> p c t", c=8), func=AF.Copy),
                             reads=[PB[half]], writes=[BmemT[mt]])
                for h in range(4):
                    for kc in range(16):
                        P.op("pe", lambda e, h=h, kc=kc: e.matmul(banks[2][:, h * 256 % 512:h * 256 % 512 + 256] if h < 2 else banks[3][:, (h - 2) * 256:(h - 2) * 256 + 256], wmk[:, kc, h * 128:(h + 1) * 128], memT[:, kc, :], start=(kc == 0), stop=(kc == 15)),
                             reads=[Bwmk] + BmemT, writes=[PB[2 + h // 2]], signal=(kc == 15))
                for hp in range(2):
                    P.op("act", lambda e, hp=hp: e.activation(out=memKT[:, hp * 2:hp * 2 + 2, :], in_=banks[2 + hp][:].rearrange("p (h m) -> p h m", h=2), func=AF.Copy), reads=[PB[2 + hp]], writes=[BmemK])
                for mt in range(2):
                    for kc in range(16):
                        P.op("pe", lambda e, mt=mt, kc=kc: e.matmul(banks[4 + mt][:], memT[:, kc, mt * 128:(mt + 1) * 128], wmv[:, kc, :], start=(kc == 0), stop=(kc == 15)),
                             reads=[Bwmv] + BmemT, writes=[PB[4 + mt]], signal=(kc == 15))
                    P.op("dve", lambda e, mt=mt: e.tensor_copy(out=memV[:, mt, :], in_=banks[4 + mt][:]), reads=[PB[4 + mt]], writes=[BmemV])
                P.barrier()
                P.flush()
            qmT = [sbt(st, "qmT%d" % k, [128, 4, 128], BF16) for k in range(2)]; Pm = sbt(st, "Pm", [128, 8, 128], BF16); oT = sbt(st, "oT", [128, 4, 128], BF16); rec = sbt(st, "recc", [128, 512], F32)
            BqmT = bufs(2)
            BPm, BoT, Brec = bufs(3)
            h3 = sbt(st, "h3", [128, D], F32); h3T = sbt(st, "h3T", [128, 16, 128], F32); Bh3, Bh3T = bufs(2)
            h3b = [sbt(st, "h3b%d" % k, [128, D], BF16) for k in range(2)]; Bh3b = bufs(2)
            lg = [sbt(st, "lg%d" % k, [128, 72], F32) for k in range(2)]; sm = sbt(st, "sm", [128, 64], F32); Blg = bufs(2); Bsm = sbufs(64)
            E1 = sbt(st, "E1", [128, 64], F32); E2 = sbt(st, "E2", [128, 64], F32); Ef = sbt(st, "Ef", [128, 64], F32); Eb = sbt(st, "Eb", [128, 64], BF16)
            Ecum = sbt(st, "Ecum", [128, 64], BF16); j64 = sbt(st, "j64", [128, 64], F32)
            BE1, BE2, BEf, BEb, BEcum, Bj64 = bufs(6)
            slf = sbt(st, "slf", [128, 2], F32); Bslf2 = sbufs(2); Bgate2 = sbufs(2); j64b = sbt(st, "j64b", [128, 64], F32); Bj64b = Buf()
            P.op("dve", lambda e: e.memset(Ecum[:], 0.0), writes=[BEcum])
            sc_m = 1.0 / math.sqrt(128.0)
            def c2_fa(i):
                k = i % 3
                kq = i % 2
                ts_ = slice(i * 128, (i + 1) * 128)
                P.dma("sp", lambda e, k=k, ts_=ts_: e.dma_start(out=xt[k][:], in_=X1[ts_, :]), "xd%d" % k, reads=[BX1[i]], writes=[Bxt[k]])
                P.op("act", lambda e, k=k: e.activation(out=junk[:], in_=xt[k][:], func=AF.Square, accum_out=stt[:, 0:1]), reads=[Bxt[k]], writes=[Bjunk, Bst[0]])
                rstd_from_ss((stt[:, 1:2], Bst[1]), stt[:, 0:1], Bst[0], stt[:, 2:3], Bst[2], D, EPS)
                P.op("dve", lambda e, k=k: e.scalar_tensor_tensor(out=hb[:], in0=xt[k][:], scalar=stt[:, 2:3], in1=gx_t[:], op0=ALU.mult, op1=ALU.mult),
                     reads=[Bxt[k], Bst[2], Bgx], writes=[Bhb])
                yield
                for half in range(2):
                    for c in range(8):
                        kc = half * 8 + c
                        P.op("pe", lambda e, c=c, kc=kc, half=half: e.transpose(out=bank_bf(half)[:, c * 128:(c + 1) * 128], in_=hb[:, kc * 128:(kc + 1) * 128], identity=identb[:]),
                             reads=[Bhb, Bident], writes=[PB[half]], signal=(c == 7))
                    P.op("act" if half == 0 else "dve", (lambda e, half=half: e.activation(out=hT[:, half * 8:(half + 1) * 8, :], in_=bank_bf(half).rearrange("p (c t) -> p c t", c=8), func=AF.Copy)) if half == 0 else
                         (lambda e, half=half: e.tensor_copy(out=hT[:, half * 8:(half + 1) * 8, :], in_=bank_bf(half).rearrange("p (c t) -> p c t", c=8))),
                         reads=[PB[half]], writes=[BhT])
                yield
                for h in range(4):
                    for kc in range(16):
                        P.op("pe", lambda e, h=h, kc=kc: e.matmul(banks[2][:, h * 128:(h + 1) * 128], wmq[:, kc, h * 128:(h + 1) * 128], hT[:, kc, :], start=(kc == 0), stop=(kc == 15)),
                             reads=[Bwmq, BhT], writes=[PB[2]], signal=(kc == 15 and h == 3))
                P.op("act", lambda e: e.activation(out=qmT[kq][:], in_=banks[2][:].rearrange("p (h t) -> p h t", h=4), func=AF.Copy), reads=[PB[2]], writes=[BqmT[kq]])
                yield

            def c2_fb(i):
                k = i % 3
                kq = i % 2
                ts_ = slice(i * 128, (i + 1) * 128)
                for h in range(4):
                    for mt in range(2):
                        c = h * 2 + mt
                        bk = 3 + c // 4
                        P.op("pe", lambda e, h=h, mt=mt, c=c, bk=bk: e.matmul(banks[bk][:, (c % 4) * 128:(c % 4 + 1) * 128], memKT[:, h, mt * 128:(mt + 1) * 128], qmT[kq][:, h, :], start=True, stop=True),
                             reads=[BmemK, BqmT[kq]], writes=[PB[bk]], signal=(c % 4 == 3))
                for hp in range(2):
                    P.op("act", lambda e, hp=hp: e.activation(out=Pm[:, hp * 4:hp * 4 + 4, :], in_=banks[3 + hp][:].rearrange("p (c t) -> p c t", c=4), func=AF.Exp, scale=sc_m),
                         reads=[PB[3 + hp]], writes=[BPm])
                yield
                for h in range(4):
                    for mt in range(2):
                        P.op("pe", lambda e, h=h, mt=mt: e.matmul(banks[3][:, h * 128:(h + 1) * 128], memV[:, mt, h * 128:(h + 1) * 128], Pm[:, h * 2 + mt, :], start=(mt == 0), stop=(mt == 1)),
                             reads=[BmemV, BPm], writes=[PB[3]], signal=False)
                    for mt in range(2):
                        P.op("pe", lambda e, h=h, mt=mt: e.matmul(banks[4][:, h * 128:(h + 1) * 128], onesb[:], Pm[:, h * 2 + mt, :], start=(mt == 0), stop=(mt == 1)),
                             reads=[Bones, BPm], writes=[PB[3], PB[4]], signal=(mt == 1 and h == 3))
                P.op("dve", lambda e: e.reciprocal(out=rec[:], in_=banks[4][:]), reads=[PB[4]], writes=[Brec])
                P.op("dve", lambda e: e.tensor_tensor(out=oT[:].rearrange("p h t -> p (h t)"), in0=banks[3][:], in1=rec[:], op=ALU.mult), reads=[PB[3], Brec], writes=[BoT])
                yield
                for cb in range(4):
                    bk = 3 if cb % 2 == 0 else 4
                    for h in range(4):
                        P.op("pe", lambda e, h=h, cb=cb, bk=bk: e.matmul(banks[bk][:], oT[:, h, :], wmo[:, h, cb * 512:(cb + 1) * 512], start=(h == 0), stop=(h == 3)),
                             reads=[BoT, Bwmo], writes=[PB[bk]], signal=(h == 3))
                    P.op("dve", lambda e, k=k, cb=cb, bk=bk: e.tensor_tensor(out=xt[k][:, cb * 512:(cb + 1) * 512], in0=banks[bk][:], in1=xt[k][:, cb * 512:(cb + 1) * 512], op=ALU.add),
                         reads=[PB[bk], Bxt[k]], writes=[Bxt[k]])
                yield
                P.dma("sp", lambda e, k=k, ts_=ts_: e.dma_start(out=X2[ts_, :], in_=xt[k][:]), "x2w%d" % k, reads=[Bxt[k]], writes=[BX2[i]])
                yield

            def c2_ba(i):
                k = i % 3
                kh = i % 2
                ts_ = slice(i * 128, (i + 1) * 128)
                P.op("act", lambda e, k=k: e.activation(out=junk[:], in_=xt[k][:], func=AF.Square, accum_out=stt[:, 3:4]), reads=[Bxt[k]], writes=[Bjunk, Bst[3]])
                rstd_from_ss((stt[:, 4:5], Bst[4]), stt[:, 3:4], Bst[3], stt[:, 5:6], Bst[5], D, EPS)
                P.op("dve", lambda e, k=k: e.scalar_tensor_tensor(out=h3[:], in0=xt[k][:], scalar=stt[:, 5:6], in1=gmoe_t[:], op0=ALU.mult, op1=ALU.mult),
                     reads=[Bxt[k], Bst[5], Bgmoe], writes=[Bh3])
                P.op("act", lambda e, k=k: e.activation(out=h3b[kh][:], in_=h3[:], func=AF.Copy), reads=[Bh3], writes=[Bh3b[kh]])
                yield
                for q4 in range(4):
                    for c in range(4):
                        kc = q4 * 4 + c
                        P.op("pe", lambda e, c=c, kc=kc, q4=q4: e.transpose(out=banks[5 + q4 % 2][:, c * 128:(c + 1) * 128], in_=h3[:, kc * 128:(kc + 1) * 128], identity=identf),
                             reads=[Bh3, Bcst], writes=[PB[5 + q4 % 2]], signal=(c == 3))
                    P.op("act" if q4 % 2 == 0 else "dve", (lambda e, q4=q4: e.activation(out=h3T[:, q4 * 4:q4 * 4 + 4, :], in_=banks[5 + q4 % 2][:].rearrange("p (c t) -> p c t", c=4), func=AF.Copy)) if q4 % 2 == 0 else
                         (lambda e, q4=q4: e.tensor_copy(out=h3T[:, q4 * 4:q4 * 4 + 4, :], in_=banks[5 + q4 % 2][:].rearrange("p (c t) -> p c t", c=4))),
                         reads=[PB[5 + q4 % 2]], writes=[Bh3T])
                yield
                for kc in range(16):
                    P.op("pe", lambda e, kc=kc: e.matmul(banks[7][:, 0:72], h3T[:, kc, :], wr_t[:, kc, :], start=(kc == 0), stop=(kc == 15)), reads=[Bh3T, Bwr], writes=[PB[7]], signal=(kc == 15))
                P.op("dve", lambda e: e.tensor_tensor(out=lg[kh][:], in0=banks[7][:, 0:72], in1=br_t[:], op=ALU.add), reads=[PB[7], Bbr], writes=[Blg[kh]])
                yield

            def c2_bb(i):
                k = i % 3
                kh = i % 2
                ts_ = slice(i * 128, (i + 1) * 128)
                Bs = Bsm
                V = lambda a, b: sm[:, a:b]
                P.op("dve", lambda e: e.max(out=V(0, 8), in_=lg[kh][:, 0:8]), reads=[Blg[kh]], writes=[Bs[0]])
                P.op("dve", lambda e: e.tensor_scalar(out=V(8, 9), in0=V(0, 1), scalar1=-1.0, scalar2=None, op0=ALU.mult), reads=[Bs[0]], writes=[Bs[8]])
                P.op("act", lambda e: e.activation(out=V(16, 24), in_=lg[kh][:, 0:8], func=AF.Exp, bias=V(8, 9), accum_out=V(9, 10)), reads=[Blg[kh], Bs[8]], writes=[Bs[16], Bs[9]])
                P.op("dve", lambda e: e.tensor_scalar(out=V(24, 32), in0=lg[kh][:, 0:8], scalar1=V(0, 1), scalar2=None, op0=ALU.is_equal), reads=[Blg[kh], Bs[0]], writes=[Bs[24]])
                yield
                P.op("dve", lambda e: e.tensor_scalar(out=V(32, 40), in0=lg[kh][:, 8:16], scalar1=V(24, 25), scalar2=None, op0=ALU.mult), reads=[Blg[kh], Bs[24]], writes=[Bs[32]])
                for g in range(1, 8):
                    P.op("dve", lambda e, g=g: e.scalar_tensor_tensor(out=V(32, 40), in0=lg[kh][:, 8 + g * 8:16 + g * 8], scalar=V(24 + g, 25 + g), in1=V(32, 40), op0=ALU.mult, op1=ALU.add),
                         reads=[Blg[kh], Bs[24], Bs[32]], writes=[Bs[32]])
                P.op("dve", lambda e: e.max(out=V(40, 48), in_=V(32, 40)), reads=[Bs[32]], writes=[Bs[40]])
                yield
                P.op("dve", lambda e: e.tensor_tensor(out=V(10, 11), in0=V(41, 42), in1=V(40, 41), op=ALU.subtract), reads=[Bs[40]], writes=[Bs[10]])
                P.op("act", lambda e: e.activation(out=V(11, 12), in_=V(10, 11), func=AF.Exp), reads=[Bs[10]], writes=[Bs[11]])
                P.op("dve", lambda e: e.scalar_tensor_tensor(out=V(12, 13), in0=V(11, 12), scalar=1.0, in1=V(9, 10), op0=ALU.add, op1=ALU.mult), reads=[Bs[11], Bs[9]], writes=[Bs[12]])
                P.op("dve", lambda e: e.reciprocal(out=V(13, 14), in_=V(12, 13)), reads=[Bs[12]], writes=[Bs[13]])
                P.op("dve", lambda e: e.tensor_tensor(out=V(14, 15), in0=V(11, 12), in1=V(13, 14), op=ALU.mult), reads=[Bs[11], Bs[13]], writes=[Bs[14]])
                yield
                P.op("dve", lambda e: e.tensor_scalar(out=V(48, 56), in0=V(32, 40), scalar1=V(40, 41), scalar2=None, op0=ALU.is_equal), reads=[Bs[32], Bs[40]], writes=[Bs[48]])
                P.op("dve", lambda e: e.tensor_scalar(out=V(56, 64), in0=V(32, 40), scalar1=V(41, 42), scalar2=None, op0=ALU.is_equal), reads=[Bs[32], Bs[40]], writes=[Bs[56]])
                ohg_b = V(24, 32).unsqueeze(2).to_broadcast([128, 8, 8])
                P.op("dve", lambda e: e.tensor_tensor(out=E1[:].rearrange("p (g j) -> p g j", g=8), in0=ohg_b, in1=V(48, 56).unsqueeze(1).to_broadcast([128, 8, 8]), op=ALU.mult),
                     reads=[Bs[24], Bs[48]], writes=[BE1])
                P.op("dve", lambda e: e.tensor_tensor(out=E2[:].rearrange("p (g j) -> p g j", g=8), in0=ohg_b, in1=V(56, 64).unsqueeze(1).to_broadcast([128, 8, 8]), op=ALU.mult),
                     reads=[Bs[24], Bs[56]], writes=[BE2])
                P.op("dve", lambda e: e.tensor_tensor(out=Eb[:], in0=E1[:], in1=E2[:], op=ALU.add), reads=[BE1, BE2], writes=[BEb])
                yield
                P.op("pe", lambda e, i=i: e.matmul(banks[7][:, 128:192], ltrib[:], Eb[:], start=True, stop=(i == 0)), reads=[Bltri, BEb], writes=[PB[7]], signal=(i == 0))
                if i > 0:
                    P.op("pe", lambda e: e.matmul(banks[7][:, 128:192], onesb[:], Ecum[:], start=False, stop=True), reads=[Bones, BEcum], writes=[PB[7]], signal=True)
                P.op("dve", lambda e: e.tensor_tensor(out=Ecum[:], in0=Ecum[:], in1=Eb[:], op=ALU.add), reads=[BEb, BEcum], writes=[BEcum])
                yield
                EK = ((E1, BE1, j64, Bj64), (E2, BE2, j64b, Bj64b))
                C0 = (16, 20)
                for stp in range(7):
                    for kk in range(2):
                        Ek, BEk, jk, Bjk = EK[kk]
                        c0 = C0[kk]
                        if stp == 0:
                            P.op("dve", lambda e, Ek=Ek, c0=c0, jk=jk: e.scalar_tensor_tensor(out=jk[:], in0=Ek[:], scalar=1.0, in1=banks[7][:, 128:192], op0=ALU.mult, op1=ALU.mult, accum_out=V(c0, c0 + 1)),
                                 reads=[BEk, PB[7]], writes=[Bjk, Bs[c0]])
                        elif stp == 1:
                            P.op("dve", lambda e, Ek=Ek, c0=c0, jk=jk: e.scalar_tensor_tensor(out=jk[:], in0=Ek[:], scalar=1.0, in1=io_t[:], op0=ALU.mult, op1=ALU.mult, accum_out=V(c0 + 1, c0 + 2)),
                                 reads=[BEk, Bio], writes=[Bjk, Bs[c0 + 1]])
                        elif stp == 2:
                            P.op("dve", lambda e, c0=c0: e.scalar_tensor_tensor(out=V(c0 + 2, c0 + 3), in0=V(c0 + 1, c0 + 2), scalar=float(CAP), in1=V(c0, c0 + 1), op0=ALU.mult, op1=ALU.add),
                                 reads=[Bs[c0], Bs[c0 + 1]], writes=[Bs[c0 + 2]])
                        elif stp == 3:
                            P.op("dve", lambda e, c0=c0: e.tensor_scalar(out=V(c0 + 3, c0 + 4), in0=V(c0, c0 + 1), scalar1=CAP - 0.5, scalar2=None, op0=ALU.is_lt), reads=[Bs[c0]], writes=[Bs[c0 + 3]])
                        elif stp == 4:
                            P.op("dve", lambda e, c0=c0: e.scalar_tensor_tensor(out=V(c0 + 2, c0 + 3), in0=V(c0 + 2, c0 + 3), scalar=dm_t[:, 0:1], in1=V(c0 + 3, c0 + 4), op0=ALU.subtract, op1=ALU.mult),
                                 reads=[Bs[c0 + 2], Bs[c0 + 3], Bdm], writes=[Bs[c0 + 2]])
                        elif stp == 5:
                            P.op("dve", lambda e, c0=c0, kk=kk: e.tensor_tensor(out=slf[:, kk:kk + 1], in0=V(c0 + 2, c0 + 3), in1=dm_t[:, 0:1], op=ALU.add), reads=[Bs[c0 + 2], Bdm], writes=[Bslf2[kk]])
                        else:
                            P.op("dve", lambda e, c0=c0, kk=kk, i=i: e.tensor_tensor(out=gates[:, i, kk:kk + 1], in0=V(13 + kk, 14 + kk), in1=V(c0 + 3, c0 + 4), op=ALU.mult),
                                 reads=[Bs[13 + kk], Bs[c0 + 3]], writes=[Bgate2[kk]])
                P.op("dve", lambda e, i=i: e.tensor_copy(out=slots[:, i, :], in_=slf[:]), reads=Bslf2 + Bgate2, writes=[Bslots[i]])
                for kk in range(2):
                    P.dma("pool", lambda e, k=k, i=i, kk=kk: e.indirect_dma_start(out=Xbuf[:, :], out_offset=bass.IndirectOffsetOnAxis(ap=slots[:, i, kk:kk + 1], axis=0), in_=h3b[kh][:], in_offset=None),
                          "scat%d" % kh, reads=[Bh3b[kh], Bslots[i]], writes=[BXb])
                yield

            def interleave(gens):
                active = [g for g in gens if g is not None]
                while active:
                    for g in list(active):
                        try:
                            next(g)
                        except StopIteration:
                            active.remove(g)

            for s in range(-2, NT + 1):
                interleave([c2_fa(s + 2) if 0 <= s + 2 < NT else None,
                            c2_fb(s + 1) if 0 <= s + 1 < NT else None,
                            c2_ba(s) if 0 <= s < NT else None,
                            c2_bb(s - 1) if 0 <= s - 1 < NT else None])
            if debug:
                P.dma("sp", lambda e: e.dma_start(out=dbgS[:, :, 0:2], in_=gates[:]), "dbg", reads=Bslots)
            P.barrier()
            P.flush()

        with contextlib.ExitStack() as st:
            Wg = [sbt(st, "Wg%d" % k, [128, 16, 512], BF16) for k in range(2)]
            Wu = [sbt(st, "Wu%d" % k, [128, 16, 512], BF16) for k in range(2)]
            Wd = [sbt(st, "Wd%d" % k, [128, 4, D], BF16) for k in range(2)]
            BWg, BWu, BWd = bufs(2), bufs(2), bufs(2)
            Xe = [sbt(st, "Xe%d" % k, [128, D], BF16) for k in range(2)]; BXe = bufs(2)
            XT = sbt(st, "XT", [128, 16, 128], BF16); BXT = Buf()
            sg = sbt(st, "sg", [128, 512], F32); Hh = sbt(st, "Hh", [128, 512], BF16); HT = sbt(st, "HT", [128, 4, 128], BF16); Bsg, BHh, BHT = bufs(3)
            Ys = [sbt(st, "Ys%d" % k, [128, D], BF16) for k in range(2)]; BYs = bufs(2)
            zt = sbt(st, "zt", [128, D], BF16); Bzt = Buf()
            P.op("dve", lambda e: e.memset(zt[:], 0.0), writes=[Bzt])
            P.dma("sp", lambda e: e.dma_start(out=Ybuf[NEXP * CAP:NSLOT, :], in_=zt[:]), "yz", reads=[Bzt], writes=[BYb])

            def load_expert(ex):
                k = ex % 2
                P.dma("pool", lambda e, k=k, ex=ex: e.dma_start(out=Wg[k][:].rearrange("p kc n -> p (kc n)"), in_=w_g[ex], max_dma_last_dim=8192), "wg%d" % k, writes=[BWg[k]])
                P.dma("pool", lambda e, k=k, ex=ex: e.dma_start(out=Wu[k][:].rearrange("p kc n -> p (kc n)"), in_=w_u[ex], max_dma_last_dim=8192), "wu%d" % k, writes=[BWu[k]])
                P.dma("pool", lambda e, k=k, ex=ex: e.dma_start(out=Wd[k][:].rearrange("p kc n -> p (kc n)"), in_=w_d[ex], max_dma_last_dim=8192), "wd%d" % k, writes=[BWd[k]])

            def load_x(gb):
                kx = gb % 2
                P.dma("sp", lambda e, kx=kx, gb=gb: e.dma_start(out=Xe[kx][:], in_=Xbuf[gb * 128:(gb + 1) * 128, :]), "xe%d" % kx, reads=[BXb], writes=[BXe[kx]])

            def stage_T(gb):
                kx = gb % 2
                for half in range(2):
                    for c in range(8):
                        kc = half * 8 + c
                        P.op("pe", lambda e, c=c, kc=kc, half=half, kx=kx: e.transpose(out=bank_bf(half)[:, c * 128:(c + 1) * 128], in_=Xe[kx][:, kc * 128:(kc + 1) * 128], identity=identb[:]),
                             reads=[BXe[kx], Bident], writes=[PB[half]], signal=(c == 7))
                    P.op("act" if half == 0 else "dve", (lambda e, half=half: e.activation(out=XT[:, half * 8:(half + 1) * 8, :], in_=bank_bf(half).rearrange("p (c t) -> p c t", c=8), func=AF.Copy)) if half == 0 else
                         (lambda e, half=half: e.tensor_copy(out=XT[:, half * 8:(half + 1) * 8, :], in_=bank_bf(half).rearrange("p (c t) -> p c t", c=8))),
                         reads=[PB[half]], writes=[BXT])

            def stage_GU(gb):
                k = (gb // NBLK) % 2
                for kc in range(16):
                    P.op("pe", lambda e, kc=kc, k=k: e.matmul(banks[2][:], XT[:, kc, :], Wg[k][:, kc, :], start=(kc == 0), stop=(kc == 15)), reads=[BXT, BWg[k]], writes=[PB[2]], signal=(kc == 15))
                for kc in range(16):
                    P.op("pe", lambda e, kc=kc, k=k: e.matmul(banks[3][:], XT[:, kc, :], Wu[k][:, kc, :], start=(kc == 0), stop=(kc == 15)), reads=[BXT, BWu[k]], writes=[PB[3]], signal=(kc == 15))
                P.op("act", lambda e: e.activation(out=sg[:], in_=banks[2][:], func=AF.Silu), reads=[PB[2]], writes=[Bsg])
                P.op("dve", lambda e: e.tensor_tensor(out=Hh[:], in0=banks[3][:], in1=sg[:], op=ALU.mult), reads=[PB[3], Bsg], writes=[BHh])

            def stage_D(gb):
                k = (gb // NBLK) % 2
                kx = gb % 2
                for c in range(4):
                    P.op("pe", lambda e, c=c: e.transpose(out=bank_bf(4)[:, c * 128:(c + 1) * 128], in_=Hh[:, c * 128:(c + 1) * 128], identity=identb[:]), reads=[BHh, Bident], writes=[PB[4]], signal=(c == 3))
                P.op("dve", lambda e: e.tensor_copy(out=HT[:], in_=bank_bf(4)[:, 0:512].rearrange("p (c t) -> p c t", c=4)), reads=[PB[4]], writes=[BHT])
                for cb in range(4):
                    bk = 5 + cb % 3
                    for c in range(4):
                        P.op("pe", lambda e, c=c, cb=cb, bk=bk, k=k: e.matmul(banks[bk][:], HT[:, c, :], Wd[k][:, c, cb * 512:(cb + 1) * 512], start=(c == 0), stop=(c == 3)),
                             reads=[BHT, BWd[k]], writes=[PB[bk]], signal=(c == 3))
                    if cb % 2 == 0:
                        P.op("act", lambda e, cb=cb, bk=bk, kx=kx: e.activation(out=Ys[kx][:, cb * 512:(cb + 1) * 512], in_=banks[bk][:], func=AF.Copy), reads=[PB[bk]], writes=[BYs[kx]])
                    else:
                        P.op("dve", lambda e, cb=cb, bk=bk, kx=kx: e.tensor_copy(out=Ys[kx][:, cb * 512:(cb + 1) * 512], in_=banks[bk][:]), reads=[PB[bk]], writes=[BYs[kx]])
                P.dma("sp", lambda e, kx=kx, gb=gb: e.dma_start(out=Ybuf[gb * 128:(gb + 1) * 128, :], in_=Ys[kx][:]), "yw%d" % kx, reads=[BYs[kx]], writes=[BYb])

            NGB = NEXP * NBLK
            load_expert(0)
            load_expert(1)
            load_x(0)
            load_x(1)
            stage_T(0)
            for gb in range(NGB):
                stage_GU(gb)
                if gb + 1 < NGB:
                    stage_T(gb + 1)
                stage_D(gb)
                if gb + 2 < NGB:
                    load_x(gb + 2)
                if gb % NBLK == NBLK - 1 and gb // NBLK + 2 < NEXP:
                    load_expert(gb // NBLK + 2)
            P.barrier()
            P.flush()

        with contextlib.ExitStack() as st:
            NX, NY = 3, 4
            gf_t = sbt(st, "gf_t", [128, D], F32); Bgf = Buf()
            P.dma("sp", lambda e: e.dma_start(out=gf_t[:], in_=gfb[:]), "c0", writes=[Bgf])
            xt = [sbt(st, "xe_%d" % k, [128, D], F32) for k in range(NX)]; Bxt = bufs(NX)
            y0 = [sbt(st, "y0_%d" % k, [128, D], BF16) for k in range(NY)]; y1 = [sbt(st, "y1_%d" % k, [128, D], BF16) for k in range(NY)]; By0, By1 = bufs(NY), bufs(NY)
            ot = [sbt(st, "ot_%d" % k, [128, D], F32) for k in range(NX)]; Bot = bufs(NX)
            junk = sbt(st, "junke", [128, D], BF16); Bjunk = Buf()
            stt = [sbt(st, "stte%d" % k, [128, 4], F32) for k in range(2)]; Bst2 = [sbufs(4), sbufs(4)]

            def e_load(i):
                kx, ky = i % NX, i % NY
                ts_ = slice(i * 128, (i + 1) * 128)
                P.dma("sp", lambda e: e.dma_start(out=xt[kx][:], in_=X2[ts_, :]), "xt%d" % kx, reads=[BX2[i]], writes=[Bxt[kx]])
                P.dma("pool", lambda e: e.indirect_dma_start(out=y0[ky][:], out_offset=None, in_=Ybuf[:, :], in_offset=bass.IndirectOffsetOnAxis(ap=slots[:, i, 0:1], axis=0)),
                      "g0_%d" % ky, reads=[BYb, Bslots[i]], writes=[By0[ky]])
                P.dma("pool", lambda e: e.indirect_dma_start(out=y1[ky][:], out_offset=None, in_=Ybuf[:, :], in_offset=bass.IndirectOffsetOnAxis(ap=slots[:, i, 1:2], axis=0)),
                      "g1_%d" % ky, reads=[BYb, Bslots[i]], writes=[By1[ky]])

            def e_comp(i):
                kx, ky = i % NX, i % NY
                st_, Bst = stt[i % 2], Bst2[i % 2]
                ts_ = slice(i * 128, (i + 1) * 128)
                P.op("dve", lambda e: e.scalar_tensor_tensor(out=xt[kx][:], in0=y0[ky][:], scalar=gates[:, i, 0:1], in1=xt[kx][:], op0=ALU.mult, op1=ALU.add),
                     reads=[By0[ky], Bslots[i], Bxt[kx]], writes=[Bxt[kx]])
                P.op("dve", lambda e: e.scalar_tensor_tensor(out=xt[kx][:], in0=y1[ky][:], scalar=gates[:, i, 1:2], in1=xt[kx][:], op0=ALU.mult, op1=ALU.add),
                     reads=[By1[ky], Bslots[i], Bxt[kx]], writes=[Bxt[kx]])
                P.op("act", lambda e: e.activation(out=junk[:], in_=xt[kx][:], func=AF.Square, accum_out=st_[:, 0:1]), reads=[Bxt[kx]], writes=[Bjunk, Bst[0]])
                rstd_from_ss((st_[:, 1:2], Bst[1]), st_[:, 0:1], Bst[0], st_[:, 2:3], Bst[2], D, EPS)
                P.op("dve", lambda e: e.scalar_tensor_tensor(out=ot[kx][:], in0=xt[kx][:], scalar=st_[:, 2:3], in1=gf_t[:], op0=ALU.mult, op1=ALU.mult),
                     reads=[Bxt[kx], Bst[2], Bgf], writes=[Bot[kx]])
                P.dma("sp", lambda e: e.dma_start(out=y[ts_, :], in_=ot[kx][:]), "yo%d" % kx, reads=[Bot[kx]])

            for i in range(min(NX, NT)):
                e_load(i)
            for i in range(NT):
                e_comp(i)
                if i + NX < NT:
                    e_load(i + NX)
            P.barrier()
            P.flush()
    return nc


def _bcast(v):
    return np.ascontiguousarray(np.broadcast_to(np.asarray(v, np.float32)[None, :], (128, v.shape[0])))


def prep_shared(inp):
    f = lambda a: np.ascontiguousarray(np.asarray(a, dtype=np.float32))
    sh = {}
    sh["g1T"] = f(inp["g_norm_mix"].reshape(16, 128).T)
    sh["w_in"] = f(inp["w_in"])
    sh["gvb"] = _bcast(inp["g_v"]); sh["bvb"] = _bcast(inp["b_v"]); sh["gab"] = _bcast(inp["g_out_gmlp"])
    sh["wsT"] = f(np.transpose(inp["w_spatial"], (2, 0, 1)))
    sh["bspT"] = f(inp["b_spatial"].T)
    sh["gqb"] = _bcast(inp["g_q_lora"]); sh["gkvb"] = _bcast(inp["g_kv_lora"])
    wq = np.asarray(inp["w_uq"], np.float32).reshape(512, 8, 192)
    sh["w_uq"] = f(np.concatenate([wq[:, :, 0:128], wq[:, :, 128:192], wq[:, :, 160:192], wq[:, :, 128:160]], axis=2))
    sh["w_ukv"] = f(inp["w_ukv"])
    sh["gm"] = f(inp["g_out_mla"].reshape(8, 128).T)
    sh["w_out"] = f(inp["w_out"])
    sh["gxb"] = _bcast(inp["g_norm_xattn"]); sh["gmemb"] = _bcast(inp["g_norm_mem"]); sh["gmoeb"] = _bcast(inp["g_norm_moe"]); sh["gfb"] = _bcast(inp["g_final"])
    sh["w_mq"] = f(inp["w_mq"]); sh["w_mk"] = f(inp["w_mk"]); sh["w_mv"] = f(inp["w_mv"]); sh["w_mo"] = f(inp["w_mo"])
    sh["wr"] = f(np.concatenate([inp["w_router_group"], inp["w_router_expert"]], axis=1))
    sh["brb"] = _bcast(np.concatenate([inp["b_router_group"], inp["b_router_expert"]]))
    def lay(w, kc):
        w = np.asarray(w, np.float32)
        e_, r_, n_ = w.shape
        return np.ascontiguousarray(w.reshape(e_, kc, 128, n_).transpose(0, 2, 1, 3)).reshape(e_, 128, kc * n_)
    sh["w_g"] = lay(inp["w_exp_gate"], 16); sh["w_u"] = lay(inp["w_exp_up"], 16); sh["w_d"] = lay(inp["w_exp_down"], 4)
    pos = np.arange(S, dtype=np.float32)
    inv = (np.float32(10000.0) ** (-np.arange(0, 64, 2, dtype=np.float32) / np.float32(64))).astype(np.float32)
    ang = (pos[:, None] * inv[None, :]).astype(np.float32)
    c, s_ = np.cos(ang).astype(np.float32), np.sin(ang).astype(np.float32)
    sh["cs_tok"] = f(np.concatenate([c, c], axis=1)); sh["sn_tok"] = f(np.concatenate([-s_, s_], axis=1))
    sh["csT"] = f(sh["cs_tok"].T); sh["snT"] = f(sh["sn_tok"].T)
    ii = np.arange(128)
    consts = np.zeros((128, 5, 128), np.float32)
    consts[:, 0, :] = np.eye(128)
    consts[:, 1, :] = (ii[:, None] <= ii[None, :])
    consts[:, 2, :] = (ii[:, None] < ii[None, :])
    consts[:, 3, :] = 1.0
    sh["consts"] = consts
    sh["iota64"] = f(np.broadcast_to(np.arange(64, dtype=np.float32)[None, :], (128, 64)))
    sh["dummyrow"] = f((NEXP * CAP + ii).reshape(128, 1))
    return sh


def prep_core(inp, b):
    xb = np.asarray(inp["x"][b], np.float32)
    return {
        "x": np.ascontiguousarray(xb),
        "xT": np.ascontiguousarray(xb.reshape(NT, 128, 16, 128).transpose(0, 3, 2, 1)),
        "mem": np.ascontiguousarray(np.asarray(inp["mem"][b], np.float32)),
    }


def kernel(**inputs):
    nc = build_program(False)
    sh = prep_shared(inputs)
    in_maps = []
    for b in range(8):
        m = dict(sh)
        m.update(prep_core(inputs, b))
        in_maps.append(m)
    res = run_bass_kernel_spmd(nc, in_maps, core_ids=list(range(8)))
    return np.stack([np.asarray(r["y"], np.float32) for r in res.results], axis=0)
```
